# Optimizing a Trainium2 kernel written in Bass

```python
import math
import jax, jax.numpy as jnp
from jax import lax
import numpy as np

D_MODEL = 2048
BATCH = 8
SEQ = 4096
DEPTH = 2

CHUNK = 64
QBLOCK = 128
N_MIXERS = 2
N_MLA_LAYERS = (DEPTH + 1) // 2
N_POOL_LAYERS = DEPTH // 2
N_HEADS = 16
Q_LORA = 512
KV_LORA = 512
NOPE_DIM = 128
ROPE_DIM = 64
V_DIM = 128
QK_HEAD = NOPE_DIM + ROPE_DIM
MLA_IN = Q_LORA + KV_LORA + ROPE_DIM
ROPE_THETA = 10000.0
POOL_WINDOWS = (2, 4, 8, 16)
N_POOL_GROUPS = len(POOL_WINDOWS)
POOL_GROUP_DIM = D_MODEL // N_POOL_GROUPS
N_GROUPS = 8
EXPERTS_PER_GROUP = 8
N_EXPERTS = N_GROUPS * EXPERTS_PER_GROUP
TOP_K_IN_GROUP = 2
D_EXPERT = 512
EXPERT_BLOCK = 256
EPS = 1e-6
NEG_INF = -1e30

kernel_name = "hybrid_mla_pool_hmoe_streaming"


def rms_norm(x, g):
    xf = x.astype(jnp.float32)
    y = xf * lax.rsqrt(jnp.mean(xf * xf, axis=-1, keepdims=True) + EPS)
    return (y * g.astype(jnp.float32)).astype(x.dtype)


def apply_rope(x, cos, sin):
    half = x.shape[-1] // 2
    xf = x.astype(jnp.float32)
    x1, x2 = xf[..., :half], xf[..., half:]
    return jnp.concatenate([x1 * cos - x2 * sin, x2 * cos + x1 * sin], axis=-1).astype(x.dtype)


def mla_mixer(h, positions, w_in, q_lat_norm, kv_lat_norm, w_q_up, w_kv_up, q_norm, k_norm, w_out):
    B, S, _ = h.shape
    c = h @ w_in
    cq = rms_norm(c[..., :Q_LORA], q_lat_norm)
    ckv = rms_norm(c[..., Q_LORA:Q_LORA + KV_LORA], kv_lat_norm)
    k_rope = c[..., Q_LORA + KV_LORA:]
    q = (cq @ w_q_up).reshape(B, S, N_HEADS, QK_HEAD)
    kv = (ckv @ w_kv_up).reshape(B, S, N_HEADS, NOPE_DIM + V_DIM)
    k_nope, v = kv[..., :NOPE_DIM], kv[..., NOPE_DIM:]
    k = jnp.concatenate(
        [k_nope, jnp.broadcast_to(k_rope[:, :, None, :], (B, S, N_HEADS, ROPE_DIM))], axis=-1)
    q = rms_norm(q, q_norm)
    k = rms_norm(k, k_norm)
    inv_freq = 1.0 / (ROPE_THETA ** (jnp.arange(0, ROPE_DIM, 2, dtype=jnp.float32) / ROPE_DIM))
    ang = positions.astype(jnp.float32)[..., None] * inv_freq
    cos = jnp.cos(ang)[:, :, None, :]
    sin = jnp.sin(ang)[:, :, None, :]
    q = jnp.concatenate([q[..., :NOPE_DIM], apply_rope(q[..., NOPE_DIM:], cos, sin)], axis=-1)
    k = jnp.concatenate([k[..., :NOPE_DIM], apply_rope(k[..., NOPE_DIM:], cos, sin)], axis=-1)
    scale = QK_HEAD ** -0.5
    outs = []
    for qb in range(S // QBLOCK):
        q0 = qb * QBLOCK
        kend = q0 + QBLOCK
        qblk = q[:, q0:kend]
        s = jnp.einsum('bqhd,bkhd->bhqk', qblk, k[:, :kend],
                       preferred_element_type=jnp.float32) * scale
        qi = q0 + jnp.arange(QBLOCK)
        kj = jnp.arange(kend)
        allowed = (kj[None, :] // CHUNK) <= (qi[:, None] // CHUNK)
        s = jnp.where(allowed[None, None], s, NEG_INF)
        p = jax.nn.softmax(s, axis=-1)
        outs.append(jnp.einsum('bhqk,bkhd->bqhd', p.astype(v.dtype), v[:, :kend]))
    o = jnp.concatenate(outs, axis=1).reshape(B, S, N_HEADS * V_DIM)
    return o @ w_out


def pool_mixer(h, w_in, w_group, scale, w_out):
    B, S, D = h.shape
    u = (h @ w_in).reshape(B, S, N_POOL_GROUPS, POOL_GROUP_DIM)
    uf = u.astype(jnp.float32)
    cs = jnp.concatenate(
        [jnp.zeros((B, 1, N_POOL_GROUPS, POOL_GROUP_DIM), jnp.float32), jnp.cumsum(uf, axis=1)],
        axis=1)
    t = jnp.arange(S)
    pooled = []
    for gi, w in enumerate(POOL_WINDOWS):
        lo = jnp.maximum(t + 1 - w, 0)
        cnt = jnp.minimum(t + 1, w).astype(jnp.float32)
        cg = cs[:, :, gi]
        pooled.append((cg[:, 1:] - cg[:, lo]) / cnt[None, :, None])
    p = jnp.stack(pooled, axis=2) - uf
    y = jnp.einsum('bsgc,gcd->bsgd', p.astype(h.dtype), w_group).reshape(B, S, D) * scale
    return y @ w_out


def hier_moe(h2, w_rg, b_rg, w_re, b_re, w_gate, w_up, w_down):
    T, D = h2.shape
    lg = jnp.einsum('td,dg->tg', h2, w_rg).astype(jnp.float32) + b_rg.astype(jnp.float32)
    pg = jax.nn.softmax(lg, axis=-1)
    g_sel = jnp.argmax(pg, axis=-1)
    p_sel = jnp.take_along_axis(pg, g_sel[:, None], axis=-1)[:, 0]
    le = jnp.einsum('td,de->te', h2, w_re).astype(jnp.float32).reshape(
        T, N_GROUPS, EXPERTS_PER_GROUP) + b_re.astype(jnp.float32)[None]
    le_sel = jnp.take_along_axis(le, g_sel[:, None, None], axis=1)[:, 0]
    qv, qi = lax.top_k(jax.nn.softmax(le_sel, axis=-1), TOP_K_IN_GROUP)
    qv = qv / jnp.sum(qv, axis=-1, keepdims=True)
    gate_w = p_sel[:, None] * qv
    eid = g_sel[:, None] * EXPERTS_PER_GROUP + qi
    A = T * TOP_K_IN_GROUP
    e_flat = eid.reshape(-1).astype(jnp.int32)
    tok_flat = jnp.repeat(jnp.arange(T, dtype=jnp.int32), TOP_K_IN_GROUP)
    w_flat = gate_w.reshape(-1)
    order = jnp.argsort(e_flat)
    e_s, tok_s, w_s = e_flat[order], tok_flat[order], w_flat[order]
    counts = jnp.bincount(e_flat, length=N_EXPERTS)
    starts = jnp.cumsum(counts) - counts
    padded = ((counts + EXPERT_BLOCK - 1) // EXPERT_BLOCK) * EXPERT_BLOCK
    pends = jnp.cumsum(padded)
    pstarts = pends - padded
    dest = pstarts[e_s] + (jnp.arange(A) - starts[e_s])
    P = A + N_EXPERTS * EXPERT_BLOCK
    n_blocks = P // EXPERT_BLOCK
    buf_tok = jnp.full((P,), T, jnp.int32).at[dest].set(tok_s)
    buf_w = jnp.zeros((P,), jnp.float32).at[dest].set(w_s)
    block_exp = jnp.minimum(
        jnp.searchsorted(pends, jnp.arange(n_blocks) * EXPERT_BLOCK, side='right'),
        N_EXPERTS - 1)
    h_pad = jnp.concatenate([h2, jnp.zeros((1, D), h2.dtype)], axis=0)

    def run_block(args):
        tok, e = args
        xb = h_pad[tok]
        return (jax.nn.silu(xb @ w_gate[e]) * (xb @ w_up[e])) @ w_down[e]

    yb = lax.map(run_block, (buf_tok.reshape(n_blocks, EXPERT_BLOCK), block_exp))
    y = yb.reshape(P, D) * buf_w[:, None].astype(yb.dtype)
    return jax.ops.segment_sum(y, buf_tok, num_segments=T + 1)[:T]


def setup_inputs(seed: int = 0) -> dict:
    key = jax.random.key(seed)
    ks = iter(jax.random.split(key, 32))
    f32 = jnp.float32

    def nrm(shape, fan_in):
        return jax.random.normal(next(ks), shape, f32) * (fan_in ** -0.5)

    def gain(shape):
        return 1.0 + 0.02 * jax.random.normal(next(ks), shape, f32)

    x = jax.random.normal(next(ks), (BATCH, SEQ, D_MODEL), f32)
    offs = jax.random.randint(next(ks), (BATCH, 1), 0, 16384, dtype=jnp.int32)
    positions = offs + jnp.arange(SEQ, dtype=jnp.int32)[None, :]
    na, npl = N_MLA_LAYERS, N_POOL_LAYERS
    return {
        "x": x,
        "positions": positions,
        "mix_norm": gain((DEPTH, D_MODEL)),
        "mla_w_in": nrm((na, D_MODEL, MLA_IN), D_MODEL),
        "mla_q_lat_norm": gain((na, Q_LORA)),
        "mla_kv_lat_norm": gain((na, KV_LORA)),
        "mla_w_q_up": nrm((na, Q_LORA, N_HEADS * QK_HEAD), Q_LORA),
        "mla_w_kv_up": nrm((na, KV_LORA, N_HEADS * (NOPE_DIM + V_DIM)), KV_LORA),
        "mla_q_norm": gain((na, QK_HEAD)),
        "mla_k_norm": gain((na, QK_HEAD)),
        "mla_w_out": nrm((na, N_HEADS * V_DIM, D_MODEL), N_HEADS * V_DIM),
        "pool_w_in": nrm((npl, D_MODEL, D_MODEL), D_MODEL),
        "pool_w_group": nrm((npl, N_POOL_GROUPS, POOL_GROUP_DIM, POOL_GROUP_DIM), POOL_GROUP_DIM),
        "pool_scale": gain((npl, D_MODEL)),
        "pool_w_out": nrm((npl, D_MODEL, D_MODEL), D_MODEL),
        "ffn_norm": gain((DEPTH, D_MODEL)),
        "moe_w_router_group": nrm((DEPTH, D_MODEL, N_GROUPS), D_MODEL),
        "moe_b_router_group": 0.01 * jax.random.normal(next(ks), (DEPTH, N_GROUPS), f32),
        "moe_w_router_expert": nrm((DEPTH, D_MODEL, N_EXPERTS), D_MODEL),
        "moe_b_router_expert": 0.01 * jax.random.normal(next(ks), (DEPTH, N_GROUPS, EXPERTS_PER_GROUP), f32),
        "moe_w_gate": nrm((DEPTH, N_EXPERTS, D_MODEL, D_EXPERT), D_MODEL),
        "moe_w_up": nrm((DEPTH, N_EXPERTS, D_MODEL, D_EXPERT), D_MODEL),
        "moe_w_down": nrm((DEPTH, N_EXPERTS, D_EXPERT, D_MODEL), D_EXPERT),
    }


def reference(x, positions, mix_norm, mla_w_in, mla_q_lat_norm, mla_kv_lat_norm, mla_w_q_up,
              mla_w_kv_up, mla_q_norm, mla_k_norm, mla_w_out, pool_w_in, pool_w_group, pool_scale,
              pool_w_out, ffn_norm, moe_w_router_group, moe_b_router_group, moe_w_router_expert,
              moe_b_router_expert, moe_w_gate, moe_w_up, moe_w_down):
    B, S, D = x.shape
    for layer in range(DEPTH):
        h = rms_norm(x, mix_norm[layer])
        if layer % N_MIXERS == 0:
            a = layer // N_MIXERS
            mixed = mla_mixer(h, positions, mla_w_in[a], mla_q_lat_norm[a], mla_kv_lat_norm[a],
                              mla_w_q_up[a], mla_w_kv_up[a], mla_q_norm[a], mla_k_norm[a],
                              mla_w_out[a])
        else:
            p = layer // N_MIXERS
            mixed = pool_mixer(h, pool_w_in[p], pool_w_group[p], pool_scale[p], pool_w_out[p])
        x = x + mixed
        h = rms_norm(x, ffn_norm[layer])
        y = hier_moe(h.reshape(B * S, D), moe_w_router_group[layer], moe_b_router_group[layer],
                     moe_w_router_expert[layer], moe_b_router_expert[layer], moe_w_gate[layer],
                     moe_w_up[layer], moe_w_down[layer])
        x = x + y.reshape(B, S, D)
    return x
```

```python
import contextlib
import numpy as np
import ml_dtypes
import concourse.bass as bass
import concourse.mybir as mybir
from concourse.bass_utils import run_bass_kernel_spmd

F32 = mybir.dt.float32
BF16 = mybir.dt.bfloat16
I32 = mybir.dt.int32
AF = mybir.ActivationFunctionType
ALU = mybir.AluOpType
AX = mybir.AxisListType

NDMA_SEMS = 24


class Reg:
    __slots__ = ("w", "r", "name")

    def __init__(self, name=""):
        self.w = None
        self.r = {}
        self.name = name


class _Cap:
    def __getattr__(self, name):
        return lambda *a, **k: (name, a, k)


_CAP = _Cap()


class Rec:
    ENGS = ("pe", "act", "dve", "pool", "sp")

    def __init__(self, nc, es):
        self.nc = nc
        self.sems = {}
        self.count = {}
        for e in self.ENGS:
            self.sems[e] = es.enter_context(nc.semaphore("s_" + e))
            self.count[e] = 0
        for i in range(NDMA_SEMS):
            k = "d%d" % i
            self.sems[k] = es.enter_context(nc.semaphore("s_" + k))
            self.count[k] = 0
        self.dma_rr = 0
        self.ops = {e: [] for e in self.ENGS}
        self.seen = {e: {} for e in self.ENGS}

    def _deps(self, eng, reads, writes):
        deps = {}

        def add(tok):
            if tok is None:
                return
            k, v = tok
            if deps.get(k, 0) < v:
                deps[k] = v
        for R in reads:
            add(R.w)
        for W in writes:
            add(W.w)
            for k, v in W.r.items():
                add((k, v))
        if eng == "pe":
            deps.pop("pe", None)
        return deps

    def _waits(self, eng, deps):
        seen = self.seen[eng]
        w = []
        for k, v in deps.items():
            if seen.get(k, 0) < v:
                seen[k] = v
                w.append((k, v))
        return w

    def _commit(self, tok, reads, writes):
        k, v = tok
        for R in reads:
            if R.r.get(k, 0) < v:
                R.r[k] = v
        for W in writes:
            W.w = tok
            W.r = {}

    def op(self, eng, fn, reads=(), writes=(), sig=True):
        deps = self._deps(eng, reads, writes)
        waits = self._waits(eng, deps)
        inc = None
        if sig:
            self.count[eng] += 1
            tok = (eng, self.count[eng])
            inc = (eng, 1)
            self._commit(tok, reads, writes)
        self.ops[eng].append((waits, fn(_CAP), inc))

    def dma(self, q, fn, reads=(), writes=(), after=()):
        deps = self._deps(q, reads, writes)
        for R in after:
            if R.w is not None and deps.get(R.w[0], 0) < R.w[1]:
                deps[R.w[0]] = R.w[1]
        k = "d%d" % self.dma_rr
        self.dma_rr = (self.dma_rr + 1) % NDMA_SEMS
        if self.count[k] > 0:
            if deps.get(k, 0) < self.count[k]:
                deps[k] = self.count[k]
        waits = self._waits(q, deps)
        self.count[k] += 16
        tok = (k, self.count[k])
        self._commit(tok, reads, writes)
        self.ops[q].append((waits, fn(_CAP), (k, 16)))

    def drain(self):
        deps = {}
        for i in range(NDMA_SEMS):
            k = "d%d" % i
            if self.count[k] > 0:
                deps[k] = self.count[k]
        waits = self._waits("sp", deps)
        if waits:
            self.ops["sp"].append((waits, None, None))

    def emit(self, name=None):
        nc = self.nc
        ops = self.ops
        self.ops = {e: [] for e in self.ENGS}
        sems = self.sems

        def replay(e, lst):
            for waits, fn, inc in lst:
                for k, v in waits:
                    e.wait_ge(sems[k], v)
                if fn is None:
                    continue
                name, a, kw = fn
                ins = getattr(e, name)(*a, **kw)
                if inc is not None:
                    ins.then_inc(sems[inc[0]], inc[1])

        with nc.Block(name) as block:
            @block.tensor
            def _(e):
                replay(e, ops["pe"])

            @block.scalar
            def _(e):
                replay(e, ops["act"])

            @block.vector
            def _(e):
                replay(e, ops["dve"])

            @block.gpsimd
            def _(e):
                replay(e, ops["pool"])

            @block.sync
            def _(e):
                replay(e, ops["sp"])


S = 4096
D = 2048
NT = S // 128
NH = 16
CAP = 256
NSLOT = 64 * CAP
TRASH = NSLOT
EPS = 1e-6
TWO_PI = 6.283185307179586
PI = 3.141592653589793


NCONV0 = 64
NCONV1 = 58


class Ctx:
    pass


def bg_issue(C, n, after=()):
    for _ in range(n):
        if not C.bg_items:
            return
        layer, ex, which = C.bg_items.pop(0)
        reg = C.r_wb[(layer, ex, which)]
        C.bg_hist.append(reg)
        deps = list(after)
        if len(C.bg_hist) > 4:
            deps.append(C.bg_hist[-5])
        if which == 0:
            src, dst, kc = C.w_gate[layer, ex], C.wgb[layer, ex], 16
        elif which == 1:
            src, dst, kc = C.w_up[layer, ex], C.wub[layer, ex], 16
        else:
            src, dst, kc = C.w_down[layer, ex], C.wdb[layer, ex], 4
        C.P.dma("pool", lambda e: e.dma_start(out=dst.rearrange("p (c n) -> p c n", c=kc),
                                              in_=src.rearrange("(c p) n -> p c n", p=128)), after=deps, writes=[reg])


def _dram(nc, name, shape, dt, kind):
    return nc.dram_tensor(name, list(shape), dt, kind=kind).ap()


def build_program(debug_out=(), phases=("a1", "a2", "a3", "m2_0", "m3_0", "l1a", "l1b", "m2_1", "m3_1")):
    nc = bass.Bass("TRN2", target_bir_lowering=False)
    C = Ctx()
    C.nc = nc
    C.debug_out = tuple(debug_out)
    I = lambda n, s, d: _dram(nc, n, s, d, "ExternalInput")
    C.x = I("x", [S, D], F32)
    C.pos = I("pos", [128, NT], I32)
    C.mix_norm = I("mix_norm", [2, D], F32)
    C.mla_w_in = I("mla_w_in", [D, 1088], F32)
    C.q_lat = I("mla_q_lat_norm", [1, 512], F32)
    C.kv_lat = I("mla_kv_lat_norm", [1, 512], F32)
    C.w_q_up = I("mla_w_q_up", [512, 3072], F32)
    C.w_kv_up = I("mla_w_kv_up", [512, 4096], F32)
    C.q_norm = I("mla_q_norm", [1, 192], F32)
    C.k_norm = I("mla_k_norm", [1, 192], F32)
    C.mla_w_out = I("mla_w_out", [D, D], F32)
    C.pool_w_in = I("pool_w_in", [D, D], F32)
    C.pool_w_group = I("pool_w_group", [4, 512, 512], F32)
    C.pool_scale = I("pool_scale", [1, D], F32)
    C.pool_w_out = I("pool_w_out", [D, D], F32)
    C.ffn_norm = I("ffn_norm", [2, D], F32)
    C.w_router = I("w_router", [2, D, 72], F32)
    C.b_router = I("b_router", [2, 72], F32)
    if any(p.startswith("m2") for p in phases):
        C.w_gate = I("moe_w_gate", [2, 64, D, 512], F32)
        C.w_up = I("moe_w_up", [2, 64, D, 512], F32)
        C.w_down = I("moe_w_down", [2, 64, 512, D], F32)
    C.c_ident = I("c_ident", [128, 128], F32)
    C.c_tri = I("c_tri", [128, 128], F32)
    C.c_invf = I("c_invf", [128, 32], F32)
    C.c_band = I("c_band", [4, 3, 128, 128], F32)
    C.c_ebase = I("c_ebase", [128, 64], F32)
    C.out = _dram(nc, "out", [S, D], F32, "ExternalOutput")

    def scratch(name, shape, dt):
        return _dram(nc, name, shape, dt, "ExternalOutput" if name in debug_out else "Internal")
    C.Qd = scratch("Qd", [S, NH * 192], BF16)
    C.Kd = scratch("Kd", [S, NH * 192], BF16)
    C.Vd = scratch("Vd", [S, NH * 128], BF16)
    C.Od = scratch("Od", [S, D], BF16)
    C.x1d = scratch("x1d", [S, D], F32)
    C.x2d = scratch("x2d", [S, D], F32)
    C.x3d = scratch("x3d", [S, D], F32)
    C.xgd = scratch("xgd", [NSLOT + 128, D], BF16)
    C.ygd = scratch("ygd", [NSLOT + 128, D], BF16)
    C.zTd = scratch("zTd", [S, D], BF16)
    C.dbg = scratch("dbg", [S, 8], F32)
    C.has_moe = any(p.startswith("m2") for p in phases)
    C.bg_items = []
    C.bg_hist = []
    C.r_wb = {}
    if C.has_moe:
        C.wgb = _dram(nc, "wgb", [2, 64, 128, 16 * 512], BF16, "Internal")
        C.wub = _dram(nc, "wub", [2, 64, 128, 16 * 512], BF16, "Internal")
        C.wdb = _dram(nc, "wdb", [2, 64, 128, 4 * D], BF16, "Internal")
        for layer, ne in ((0, NCONV0), (1, NCONV1)):
            for ex in range(ne):
                for which in range(3):
                    C.bg_items.append((layer, ex, which))
                    C.r_wb[(layer, ex, which)] = Reg("wb")

    with contextlib.ExitStack() as es:
        P = Rec(nc, es)
        C.P = P
        C.ps = [es.enter_context(nc.psum_tensor("ps%d" % i, [128, 512], F32)) for i in range(8)]
        C.psr = [Reg("ps%d" % i) for i in range(8)]
        C.ps_rr = 0
        C.tr_rr = 0
        C.slot_i = es.enter_context(nc.sbuf_tensor("slot_i", [128, NT, 2], I32))
        C.gate_w = es.enter_context(nc.sbuf_tensor("gate_w", [128, NT, 2], F32))
        C.r_slot = [Reg() for _ in range(NT)]
        for nm in ("x", "Qd", "Kd", "Vd", "Od", "x1d", "x2d", "x3d", "zTd", "out"):
            setattr(C, "r_" + nm, [Reg(nm + str(i)) for i in range(NT)])
        C.r_xg = Reg("xg")
        C.r_ygt = Reg("ygt")
        C.r_xgt = [Reg("xgt%d" % i) for i in range(NT)]
        C.r_yg = [Reg("yg%d" % e) for e in range(64)]
        C.r_xge = [Reg("xge%d" % e) for e in range(64)]
        for ph in phases:
            with contextlib.ExitStack() as pes:
                if ph == "a1":
                    phase_a1(C, pes)
                elif ph == "a2":
                    phase_a2(C, pes)
                elif ph == "a3":
                    phase_proj_moe_front(C, pes, layer=0)
                elif ph == "m2_0":
                    phase_experts(C, pes, layer=0)
                elif ph == "m3_0":
                    phase_combine(C, pes, layer=0)
                elif ph == "l1a":
                    phase_pool_a(C, pes)
                elif ph == "l1b":
                    phase_proj_moe_front(C, pes, layer=1)
                elif ph == "m2_1":
                    phase_experts(C, pes, layer=1)
                elif ph == "m3_1":
                    phase_combine(C, pes, layer=1)
                P.drain()
                P.emit(ph)
    return nc


def next_bank(C):
    pool = getattr(C, "bank_pool", None) or list(range(8))
    b = pool[C.ps_rr % len(pool)]
    C.ps_rr += 1
    return C.ps[b], C.psr[b]


_UNIQ = [0]


def sb(pes, nc, name, shape, dt):
    _UNIQ[0] += 1
    return pes.enter_context(nc.sbuf_tensor("%s_u%d" % (name, _UNIQ[0]), list(shape), dt))


def load_bcast(C, q, dst, src_row, n, reg):
    C.P.dma(q, lambda e: e.dma_start(out=dst, in_=src_row.broadcast_to([128, n])), writes=[reg])


def load_w_cast(C, dst, src, reg, kc):
    n = src.shape[1]
    if n <= 2048:
        for c0 in range(0, kc, 8):
            c1 = min(kc, c0 + 8)
            C.P.dma("pool", lambda e, c0=c0, c1=c1: e.dma_start(
                out=dst[:, c0:c1, :], in_=src[c0 * 128:c1 * 128, :].rearrange("(c p) n -> p c n", p=128)), writes=[reg])
    else:
        for c in range(kc):
            C.P.dma("pool", lambda e, c=c: e.dma_start(
                out=dst[:, c, :], in_=src[c * 128:(c + 1) * 128, :], max_dma_last_dim=8192), writes=[reg])


def transposes_bf16(C, src_fn, n, dst, dst_reg, src_regs, width=128, evac=("act", "dve"), group=8, regions=None):
    P = C.P
    for gi, g0 in enumerate(range(0, n, group)):
        g1 = min(n, g0 + group)
        if regions is None:
            bank, breg = next_bank(C)
            bv = bank[:].bitcast(BF16).rearrange("p (j t) -> p j t", t=128)
        else:
            bv, breg = regions[C.tr_rr % len(regions)]
            C.tr_rr += 1
        for j in range(g0, g1):
            P.op("pe", lambda e: e.transpose(out=bv[:width, j - g0, :], in_=src_fn(j), identity=C.identb[:]),
                 reads=list(src_regs) + [C.r_ident], writes=[breg], sig=(j == g1 - 1))
        en = evac[gi % len(evac)]
        if en == "act":
            P.op("act", lambda e: e.activation(out=dst[:width, g0:g1, :], in_=bv[:width, 0:g1 - g0, :], func=AF.Copy),
                 reads=[breg], writes=[dst_reg])
        else:
            P.op("dve", lambda e: e.tensor_copy(out=dst[:width, g0:g1, :], in_=bv[:width, 0:g1 - g0, :]),
                 reads=[breg], writes=[dst_reg])


def transposes_bf16_gen(C, src_fn, n, dst, dst_reg, src_regs, width=128, evac=("act", "dve"), group=8, regions=None):
    P = C.P
    for gi, g0 in enumerate(range(0, n, group)):
        g1 = min(n, g0 + group)
        if regions is None:
            bank, breg = next_bank(C)
            bv = bank[:].bitcast(BF16).rearrange("p (j t) -> p j t", t=128)
        else:
            bv, breg = regions[C.tr_rr % len(regions)]
            C.tr_rr += 1
        for j in range(g0, g1):
            P.op("pe", lambda e: e.transpose(out=bv[:width, j - g0, :], in_=src_fn(j), identity=C.identb[:]),
                 reads=list(src_regs) + [C.r_ident], writes=[breg], sig=(j == g1 - 1))
        en = evac[gi % len(evac)]
        if en == "act":
            P.op("act", lambda e: e.activation(out=dst[:width, g0:g1, :], in_=bv[:width, 0:g1 - g0, :], func=AF.Copy),
                 reads=[breg], writes=[dst_reg])
        else:
            P.op("dve", lambda e: e.tensor_copy(out=dst[:width, g0:g1, :], in_=bv[:width, 0:g1 - g0, :]),
                 reads=[breg], writes=[dst_reg])
        yield


def rstd_from_ss(C, ss, rs, n, reg_ss, reg_rs, width):
    P = C.P
    P.op("act", lambda e: e.activation(out=rs, in_=ss, func=AF.Sqrt, scale=1.0 / n, bias=C.eps_t[:, 0:1]),
         reads=[reg_ss, C.r_const], writes=[reg_rs])
    P.op("dve", lambda e: e.reciprocal(out=rs, in_=rs), reads=[reg_rs], writes=[reg_rs])


def interleave(genB, genA, ratio):
    k = 0
    for _ in genB:
        k += 1
        if genA is not None and k % ratio == 0:
            if next(genA, "end") == "end":
                genA = None
    if genA is not None:
        for _ in genA:
            pass


def load_consts(C, pes):
    nc, P = C.nc, C.P
    C.identf = sb(pes, nc, "identf", [128, 128], F32)
    C.identb = sb(pes, nc, "identb", [128, 128], BF16)
    C.eps_t = sb(pes, nc, "eps_t", [128, 1], F32)
    C.r_ident = Reg("ident")
    C.r_const = Reg("const")
    P.dma("sp", lambda e: e.dma_start(out=C.identf[:], in_=C.c_ident), writes=[C.r_ident])
    P.op("dve", lambda e: e.tensor_copy(out=C.identb[:], in_=C.identf[:]), reads=[C.r_ident], writes=[C.r_ident])
    P.op("dve", lambda e: e.memset(C.eps_t[:], EPS), writes=[C.r_const])


def phase_a1(C, pes):
    nc, P = C.nc, C.P
    load_consts(C, pes)
    w_in = sb(pes, nc, "a1_w_in", [128, 16, 1088], BF16)
    wq = sb(pes, nc, "a1_wq", [128, 4, 3072], BF16)
    wkv = sb(pes, nc, "a1_wkv", [128, 4, 4096], BF16)
    g_mix = sb(pes, nc, "a1_gmix", [128, D], F32)
    g_lat = sb(pes, nc, "a1_glat", [128, 1024], F32)
    g_qn = sb(pes, nc, "a1_gqn", [128, 192], F32)
    g_kn = sb(pes, nc, "a1_gkn", [128, 192], F32)
    invf = sb(pes, nc, "a1_invf", [128, 32], F32)
    pos_i = sb(pes, nc, "a1_posi", [128, NT], I32)
    pos_f = sb(pes, nc, "a1_posf", [128, NT], F32)
    NB = 2
    xt = [sb(pes, nc, "a1_xt%d" % i, [128, D], F32) for i in range(NB)]
    r_xt = [Reg() for _ in range(NB)]
    ang = xt[1][:, 0:1024].rearrange("p (a b) -> p a b", b=32)
    kk_f = xt[1][:, 1024:2048].rearrange("p (a b) -> p a b", b=32)
    kk_i = xt[0][:, 0:1024].bitcast(I32).rearrange("p (a b) -> p a b", b=32)
    msk = xt[0][:, 1024:2048].rearrange("p (a b) -> p a b", b=32)
    sin_t = sb(pes, nc, "a1_sin", [128, NT, 32], F32)
    cos_t = sb(pes, nc, "a1_cos", [128, NT, 32], F32)
    r_w = Reg("a1w")
    r_g = Reg("a1g")
    r_rope = Reg("rope")
    load_w_cast(C, w_in, C.mla_w_in, r_w, 16)
    load_bcast(C, "sp", g_mix[:], C.mix_norm[0:1, :], D, r_g)
    load_bcast(C, "sp", g_lat[:, 0:512], C.q_lat[0:1, :], 512, r_g)
    load_bcast(C, "sp", g_lat[:, 512:1024], C.kv_lat[0:1, :], 512, r_g)
    load_bcast(C, "sp", g_qn[:], C.q_norm[0:1, :], 192, r_g)
    load_bcast(C, "sp", g_kn[:], C.k_norm[0:1, :], 192, r_g)
    P.dma("sp", lambda e: e.dma_start(out=invf[:], in_=C.c_invf), writes=[r_rope])
    P.dma("sp", lambda e: e.dma_start(out=pos_i[:], in_=C.pos), writes=[r_rope])
    load_w_cast(C, wq, C.w_q_up, r_w, 4)
    load_w_cast(C, wkv, C.w_kv_up, r_w, 4)
    P.op("dve", lambda e: e.tensor_copy(out=pos_f[:], in_=pos_i[:]), reads=[r_rope], writes=[r_rope, r_xt[0], r_xt[1]])
    P.op("dve", lambda e: e.tensor_tensor(out=ang[:], in0=pos_f[:, :].unsqueeze(2).broadcast_to([128, NT, 32]),
                                          in1=invf[:, :].unsqueeze(1).broadcast_to([128, NT, 32]), op=ALU.mult),
         reads=[r_rope], writes=[r_rope, r_xt[0], r_xt[1]])
    P.op("dve", lambda e: e.tensor_scalar(out=kk_f[:], in0=ang[:], scalar1=1.0 / TWO_PI, scalar2=0.5, op0=ALU.mult, op1=ALU.add),
         reads=[r_rope], writes=[r_rope, r_xt[0], r_xt[1]])
    P.op("dve", lambda e: e.tensor_copy(out=kk_i[:], in_=kk_f[:]), reads=[r_rope], writes=[r_rope, r_xt[0], r_xt[1]])
    P.op("dve", lambda e: e.tensor_copy(out=kk_f[:], in_=kk_i[:]), reads=[r_rope], writes=[r_rope, r_xt[0], r_xt[1]])
    C1 = 6.28125
    C2 = TWO_PI - C1
    P.op("dve", lambda e: e.scalar_tensor_tensor(out=ang[:], in0=kk_f[:], scalar=-C1, in1=ang[:], op0=ALU.mult, op1=ALU.add),
         reads=[r_rope], writes=[r_rope, r_xt[0], r_xt[1]])
    P.op("dve", lambda e: e.scalar_tensor_tensor(out=ang[:], in0=kk_f[:], scalar=-C2, in1=ang[:], op0=ALU.mult, op1=ALU.add),
         reads=[r_rope], writes=[r_rope, r_xt[0], r_xt[1]])
    def wrap(buf, lo, hi):
        if lo:
            P.op("dve", lambda e: e.tensor_single_scalar(out=msk[:], in_=buf[:], scalar=-PI, op=ALU.is_lt), reads=[r_rope], writes=[r_rope, r_xt[0], r_xt[1]])
            P.op("dve", lambda e: e.scalar_tensor_tensor(out=buf[:], in0=msk[:], scalar=TWO_PI, in1=buf[:], op0=ALU.mult, op1=ALU.add), reads=[r_rope], writes=[r_rope, r_xt[0], r_xt[1]])
        if hi:
            P.op("dve", lambda e: e.tensor_single_scalar(out=msk[:], in_=buf[:], scalar=PI, op=ALU.is_gt), reads=[r_rope], writes=[r_rope, r_xt[0], r_xt[1]])
            P.op("dve", lambda e: e.scalar_tensor_tensor(out=buf[:], in0=msk[:], scalar=-TWO_PI, in1=buf[:], op0=ALU.mult, op1=ALU.add), reads=[r_rope], writes=[r_rope, r_xt[0], r_xt[1]])
    wrap(ang, True, True)
    P.op("act", lambda e: e.activation(out=sin_t[:], in_=ang[:], func=AF.Sin), reads=[r_rope], writes=[r_rope, r_xt[0], r_xt[1]])
    P.op("dve", lambda e: e.tensor_scalar(out=ang[:], in0=ang[:], scalar1=PI / 2, scalar2=None, op0=ALU.add), reads=[r_rope], writes=[r_rope, r_xt[0], r_xt[1]])
    wrap(ang, False, True)
    P.op("act", lambda e: e.activation(out=cos_t[:], in_=ang[:], func=AF.Sin), reads=[r_rope], writes=[r_rope, r_xt[0], r_xt[1]])

    junk = sb(pes, nc, "a1_junk", [128, D], BF16)
    r_junk = Reg()
    hb2 = [sb(pes, nc, "a1_hb%d" % i, [128, D], BF16) for i in range(2)]
    r_hb2 = [Reg() for _ in range(2)]
    hT2 = [sb(pes, nc, "a1_hT%d" % i, [128, 16, 128], BF16) for i in range(2)]
    r_hT2 = [Reg() for _ in range(2)]
    cn2 = [sb(pes, nc, "a1_cn%d" % i, [128, 1024], BF16) for i in range(2)]
    r_cn2 = [Reg() for _ in range(2)]
    cT2 = [sb(pes, nc, "a1_cT%d" % i, [128, 8, 128], BF16) for i in range(2)]
    r_cT2 = [Reg() for _ in range(2)]
    st2 = [sb(pes, nc, "a1_st%d" % i, [128, 64], F32) for i in range(2)]
    r_st2 = [[Reg() for _ in range(4)] for _ in range(2)]
    ssq2 = [sb(pes, nc, "a1_ssq%d" % i, [128, 16], F32) for i in range(2)]
    rq2 = [sb(pes, nc, "a1_rq%d" % i, [128, 16], F32) for i in range(2)]
    ssk2 = [sb(pes, nc, "a1_ssk%d" % i, [128, 16], F32) for i in range(2)]
    rk2 = [sb(pes, nc, "a1_rk%d" % i, [128, 16], F32) for i in range(2)]
    r_sq2 = [[Reg() for _ in range(8)] for _ in range(2)]
    r_sk2 = [[Reg() for _ in range(8)] for _ in range(2)]
    krg2 = [sb(pes, nc, "a1_krg%d" % i, [128, 64], F32) for i in range(2)]
    krr2 = [sb(pes, nc, "a1_krr%d" % i, [128, 64], F32) for i in range(2)]
    tA1 = sb(pes, nc, "a1_tA", [128, 16, 64], F32)
    tB1 = sb(pes, nc, "a1_tB", [128, 16, 64], F32)
    tA2 = [tA1, tA1]
    tB2 = [tB1, tB1]
    tK2 = [sb(pes, nc, "a1_tK%d" % i, [128, 2, 64], F32) for i in range(2)]
    r_kr2 = [Reg() for _ in range(2)]
    r_t1 = Reg()
    r_t2 = [r_t1, r_t1]
    r_tk2 = [Reg() for _ in range(2)]
    qn = [sb(pes, nc, "a1_qn%d" % i, [128, 16, 192], BF16) for i in range(NB)]
    kn = [sb(pes, nc, "a1_kn%d" % i, [128, 16, 192], BF16) for i in range(NB)]
    vv = [sb(pes, nc, "a1_vv%d" % i, [128, 16, 128], BF16) for i in range(NB)]
    r_qn = [Reg() for _ in range(NB)]
    r_kn = [Reg() for _ in range(NB)]
    r_vv = [Reg() for _ in range(NB)]

    def load_x(i):
        b = i % NB
        P.dma("sp", lambda e: e.dma_start(out=xt[b][:], in_=C.x[i * 128:(i + 1) * 128, :]), reads=[C.r_x[i]], writes=[r_xt[b]])

    def stageA(i):
        b = i % NB
        if i + 1 < NT:
            load_x(i + 1)
        X = xt[b]
        hb, r_hb, hT, r_hT, cn, r_cn, cT, r_cT, st = hb2[b], r_hb2[b], hT2[b], r_hT2[b], cn2[b], r_cn2[b], cT2[b], r_cT2[b], st2[b]
        r_st, r_stl, r_str = r_st2[b][0], r_st2[b][1], r_st2[b][2]
        ssq, rq, ssk, rk, r_sq, r_sk = ssq2[b], rq2[b], ssk2[b], rk2[b], r_sq2[b], r_sk2[b]
        krg, krr, tA, tB, tK, r_kr, r_t, r_tk = krg2[b], krr2[b], tA2[b], tB2[b], tK2[b], r_kr2[b], r_t2[b], r_tk2[b]
        P.op("act", lambda e, X=X: e.activation(out=junk[:], in_=X[:], func=AF.Square, accum_out=st[:, 0:1]), reads=[r_xt[b]], writes=[r_st])
        rstd_from_ss(C, st[:, 0:1], st[:, 1:2], D, r_st, r_st, 1)
        P.op("dve", lambda e, X=X: e.scalar_tensor_tensor(out=hb[:], in0=X[:], scalar=st[:, 1:2], in1=g_mix[:], op0=ALU.mult, op1=ALU.mult),
             reads=[r_xt[b], r_st, r_g], writes=[r_hb])
        yield
        transposes_bf16(C, lambda j: hb[:, j * 128:(j + 1) * 128], 16, hT, r_hT, [r_hb])
        yield
        slabs = [(0, 512), (512, 512), (1024, 64)]
        cb = []
        for (o, n) in slabs:
            bank, breg = next_bank(C)
            cb.append((bank, breg))
            for c in range(16):
                P.op("pe", lambda e, bank=bank, c=c, o=o, n=n: e.matmul(out=bank[:, 0:n], lhsT=hT[:, c, :], rhs=w_in[:, c, o:o + n], start=(c == 0), stop=(c == 15)),
                     reads=[r_hT, r_w], writes=[breg], sig=(c == 15))
            yield
        for s_ in range(2):
            bank, breg = cb[s_]
            P.op("act", lambda e, bank=bank, s_=s_: e.activation(out=junk[:, 0:512], in_=bank[:, :], func=AF.Square, accum_out=st[:, 2 + s_:3 + s_]),
                 reads=[breg], writes=[r_stl])
        rstd_from_ss(C, st[:, 2:4], st[:, 4:6], 512, r_stl, r_stl, 2)
        for s_ in range(2):
            bank, breg = cb[s_]
            P.op("dve", lambda e, bank=bank, s_=s_: e.scalar_tensor_tensor(out=cn[:, s_ * 512:(s_ + 1) * 512], in0=bank[:, :], scalar=st[:, 4 + s_:5 + s_],
                                                                           in1=g_lat[:, s_ * 512:(s_ + 1) * 512], op0=ALU.mult, op1=ALU.mult),
                 reads=[breg, r_stl, r_g], writes=[r_cn])
        yield
        bank, breg = cb[2]
        P.op("act", lambda e, bank=bank: e.activation(out=junk[:, 0:64], in_=bank[:, 0:64], func=AF.Square, accum_out=st[:, 6:7]),
             reads=[breg], writes=[r_str])
        P.op("dve", lambda e, bank=bank: e.tensor_tensor(out=krg[:], in0=bank[:, 0:64], in1=g_kn[:, 128:192], op=ALU.mult),
             reads=[breg, r_g], writes=[r_kr])
        cosb = cos_t[:, i, :].unsqueeze(1).broadcast_to([128, 2, 32])
        sinb = sin_t[:, i, :].unsqueeze(1).broadcast_to([128, 2, 32])
        krg3 = krg[:, :].rearrange("p (t d) -> p t d", t=2)
        tA0 = tK[:, 0, :].rearrange("p (t d) -> p t d", t=2)
        tB0 = tK[:, 1, :].rearrange("p (t d) -> p t d", t=2)
        P.op("dve", lambda e: e.tensor_tensor(out=tA0, in0=krg3, in1=cosb, op=ALU.mult), reads=[r_kr, r_rope], writes=[r_tk])
        P.op("dve", lambda e: e.tensor_tensor(out=tB0, in0=krg3, in1=sinb, op=ALU.mult), reads=[r_kr, r_rope], writes=[r_tk])
        P.op("dve", lambda e: e.tensor_tensor(out=krr[:, 0:32], in0=tK[:, 0, 0:32], in1=tK[:, 1, 32:64], op=ALU.subtract), reads=[r_tk], writes=[r_kr])
        P.op("dve", lambda e: e.tensor_tensor(out=krr[:, 32:64], in0=tK[:, 0, 32:64], in1=tK[:, 1, 0:32], op=ALU.add), reads=[r_tk], writes=[r_kr])
        yield
        transposes_bf16(C, lambda j: cn[:, j * 128:(j + 1) * 128], 8, cT, r_cT, [r_cn])

    def stageB(i):
        b = i % NB
        X = xt[b]
        hb, r_hb, hT, r_hT, cn, r_cn, cT, r_cT, st = hb2[b], r_hb2[b], hT2[b], r_hT2[b], cn2[b], r_cn2[b], cT2[b], r_cT2[b], st2[b]
        r_st, r_stl, r_str = r_st2[b][0], r_st2[b][1], r_st2[b][2]
        ssq, rq, ssk, rk, r_sq, r_sk = ssq2[b], rq2[b], ssk2[b], rk2[b], r_sq2[b], r_sk2[b]
        krg, krr, tA, tB, tK, r_kr, r_t, r_tk = krg2[b], krr2[b], tA2[b], tB2[b], tK2[b], r_kr2[b], r_t2[b], r_tk2[b]
        Q, K, V = qn[b], kn[b], vv[b]
        bg_issue(C, 2, after=[r_cT])
        for s_ in range(8):
            bank, breg = next_bank(C)
            for c in range(4):
                P.op("pe", lambda e, bank=bank, c=c, s_=s_: e.matmul(out=bank[:, 0:384], lhsT=cT[:, c, :], rhs=wq[:, c, s_ * 384:(s_ + 1) * 384], start=(c == 0), stop=(c == 3)),
                     reads=[r_cT, r_w], writes=[breg], sig=(c == 3))
            for hh in range(2):
                hd = 2 * s_ + hh
                P.op("act", lambda e, bank=bank, hh=hh, hd=hd: e.activation(out=junk[:, 0:192], in_=bank[:, hh * 192:(hh + 1) * 192], func=AF.Square, accum_out=ssq[:, hd:hd + 1]),
                     reads=[breg], writes=[r_sq[s_]])
            rstd_from_ss(C, ssq[:, 2 * s_:2 * s_ + 2], rq[:, 2 * s_:2 * s_ + 2], 192, r_sq[s_], r_sq[s_], 2)
            for hh in range(2):
                hd = 2 * s_ + hh
                P.op("dve", lambda e, bank=bank, hh=hh, hd=hd, Q=Q: e.scalar_tensor_tensor(out=Q[:, hd, :], in0=bank[:, hh * 192:(hh + 1) * 192], scalar=rq[:, hd:hd + 1],
                                                                                          in1=g_qn[:], op0=ALU.mult, op1=ALU.mult),
                     reads=[breg, r_sq[s_], r_g], writes=[r_qn[b]])
            yield
        cos16 = cos_t[:, i, :].unsqueeze(1).unsqueeze(1).broadcast_to([128, 16, 2, 32])
        sin16 = sin_t[:, i, :].unsqueeze(1).unsqueeze(1).broadcast_to([128, 16, 2, 32])
        qr4 = Q[:, :, 128:192].rearrange("p h (t d) -> p h t d", t=2)
        tA4 = tA[:, :, :].rearrange("p h (t d) -> p h t d", t=2)
        tB4 = tB[:, :, :].rearrange("p h (t d) -> p h t d", t=2)
        P.op("dve", lambda e: e.tensor_tensor(out=tA4, in0=qr4, in1=cos16, op=ALU.mult), reads=[r_qn[b], r_rope], writes=[r_t])
        P.op("dve", lambda e: e.tensor_tensor(out=tB4, in0=qr4, in1=sin16, op=ALU.mult), reads=[r_qn[b], r_rope], writes=[r_t])
        P.op("dve", lambda e, Q=Q: e.tensor_tensor(out=Q[:, :, 128:160], in0=tA[:, :, 0:32], in1=tB[:, :, 32:64], op=ALU.subtract), reads=[r_t], writes=[r_qn[b]])
        P.op("dve", lambda e, Q=Q: e.tensor_tensor(out=Q[:, :, 160:192], in0=tA[:, :, 32:64], in1=tB[:, :, 0:32], op=ALU.add), reads=[r_t], writes=[r_qn[b]])
        P.dma("sp", lambda e, Q=Q, i=i: e.dma_start(out=C.Qd[i * 128:(i + 1) * 128, :], in_=Q[:, :, :].rearrange("p h d -> p (h d)")),
              reads=[r_qn[b]], writes=[C.r_Qd[i]])
        yield
        for s_ in range(8):
            bank, breg = next_bank(C)
            for c in range(4):
                P.op("pe", lambda e, bank=bank, c=c, s_=s_: e.matmul(out=bank[:, :], lhsT=cT[:, 4 + c, :], rhs=wkv[:, c, s_ * 512:(s_ + 1) * 512], start=(c == 0), stop=(c == 3)),
                     reads=[r_cT, r_w], writes=[breg], sig=(c == 3))
            b4 = bank[:, :].rearrange("p (h t d) -> p h t d", h=2, t=2)
            for hh in range(2):
                hd = 2 * s_ + hh
                P.op("act", lambda e, b4=b4, hh=hh, hd=hd: e.activation(out=junk[:, 0:128], in_=b4[:, hh, 0, :], func=AF.Square, accum_out=ssk[:, hd:hd + 1]),
                     reads=[breg], writes=[r_sk[s_]])
            P.op("act", lambda e, b4=b4, s_=s_, V=V: e.activation(out=V[:, 2 * s_:2 * s_ + 2, :], in_=b4[:, :, 1, :], func=AF.Copy), reads=[breg], writes=[r_vv[b]])
            P.op("dve", lambda e, s_=s_: e.tensor_scalar(out=ssk[:, 2 * s_:2 * s_ + 2], in0=ssk[:, 2 * s_:2 * s_ + 2], scalar1=st[:, 6:7], scalar2=None, op0=ALU.add),
                 reads=[r_sk[s_], r_str], writes=[r_sk[s_]])
            rstd_from_ss(C, ssk[:, 2 * s_:2 * s_ + 2], rk[:, 2 * s_:2 * s_ + 2], 192, r_sk[s_], r_sk[s_], 2)
            for hh in range(2):
                hd = 2 * s_ + hh
                P.op("dve", lambda e, b4=b4, hh=hh, hd=hd, K=K: e.scalar_tensor_tensor(out=K[:, hd, 0:128], in0=b4[:, hh, 0, :], scalar=rk[:, hd:hd + 1],
                                                                                      in1=g_kn[:, 0:128], op0=ALU.mult, op1=ALU.mult),
                     reads=[breg, r_sk[s_], r_g], writes=[r_kn[b]])
            yield
        P.op("dve", lambda e: e.tensor_tensor(out=K[:, :, 128:192], in0=krr[:, :].unsqueeze(1).broadcast_to([128, 16, 64]),
                                              in1=rk[:, :].unsqueeze(2).broadcast_to([128, 16, 64]), op=ALU.mult),
             reads=[r_kr] + list(r_sk), writes=[r_kn[b]])
        P.dma("sp", lambda e, K=K, i=i: e.dma_start(out=C.Kd[i * 128:(i + 1) * 128, :], in_=K[:, :, :].rearrange("p h d -> p (h d)")),
              reads=[r_kn[b]], writes=[C.r_Kd[i]])
        P.dma("sp", lambda e, V=V, i=i: e.dma_start(out=C.Vd[i * 128:(i + 1) * 128, :], in_=V[:, :, :].rearrange("p h d -> p (h d)")),
              reads=[r_vv[b]], writes=[C.r_Vd[i]])

    load_x(0)
    for _ in stageA(0):
        pass
    for i in range(NT):
        interleave(stageB(i), stageA(i + 1) if i + 1 < NT else None, 2)


A2_SPLIT = 96
A2_REGIONS = False


def phase_a2(C, pes):
    nc, P = C.nc, C.P
    load_consts(C, pes)
    tb = C.ps[7][:].bitcast(BF16).rearrange("p (r j t) -> p r j t", r=2, t=128)
    tr_regions = [(tb[:, 0], Reg("trh0")), (tb[:, 1], Reg("trh1"))]
    if not A2_REGIONS:
        C.bank_pool = [7]
    Qh = [sb(pes, nc, "a2_Q%d" % i, [128, NT, 192], BF16) for i in range(2)]
    Kh = [sb(pes, nc, "a2_K%d" % i, [128, NT, 192], BF16) for i in range(2)]
    Vh = [sb(pes, nc, "a2_V%d" % i, [128, NT, 132], BF16) for i in range(2)]
    W1 = A2_SPLIT
    W2 = 192 - W1
    QTn = [sb(pes, nc, "a2_QTn%d" % i, [W1, NT, 128], BF16) for i in range(2)]
    QTr = [sb(pes, nc, "a2_QTr%d" % i, [W2, NT, 128], BF16) for i in range(2)]
    KTn = [sb(pes, nc, "a2_KTn%d" % i, [W1, NT, 128], BF16) for i in range(2)]
    KTr = [sb(pes, nc, "a2_KTr%d" % i, [W2, NT, 128], BF16) for i in range(2)]
    Oh = [sb(pes, nc, "a2_O%d" % i, [128, NT, 128], BF16) for i in range(2)]
    NR = 6
    PT = [sb(pes, nc, "a2_PT%d" % i, [128, 512], BF16) for i in range(NR)]
    ND = 6
    PTd = [sb(pes, nc, "a2_PTd%d" % i, [128, 512], BF16) for i in range(ND)]
    r_PTd = [Reg() for _ in range(ND)]
    for i in range(ND):
        P.op("pool", lambda e: e.memset(PTd[i][:], 0.0), writes=[r_PTd[i]])
    dctr = [0]
    rsum = sb(pes, nc, "a2_rs", [128, 8], F32)
    r_Q = [Reg() for _ in range(2)]
    r_K = [Reg() for _ in range(2)]
    r_V = [Reg() for _ in range(2)]
    r_QT = [Reg() for _ in range(2)]
    r_KT = [Reg() for _ in range(2)]
    r_O = [Reg() for _ in range(2)]
    r_PT = [Reg() for _ in range(NR)]
    r_rs = [Reg() for _ in range(8)]
    NS = 3
    Sb = [C.ps[0], C.ps[1], C.ps[2]]
    r_S = [C.psr[0], C.psr[1], C.psr[2]]
    acc = [C.ps[3 + j] for j in range(4)]
    r_acc = [C.psr[3 + j] for j in range(4)]
    for b in range(2):
        P.op("pool", lambda e: e.memset(Vh[b][:, :, 128:129], 1.0), writes=[r_V[b]])
    scale = 192.0 ** -0.5

    def prologue(h):
        b = h % 2
        P.dma("sp", lambda e: e.dma_start(out=Qh[b][:], in_=C.Qd[:, h * 192:(h + 1) * 192].rearrange("(i p) d -> p i d", p=128)),
              reads=C.r_Qd, writes=[r_Q[b]])
        P.dma("sp", lambda e: e.dma_start(out=Kh[b][:], in_=C.Kd[:, h * 192:(h + 1) * 192].rearrange("(i p) d -> p i d", p=128)),
              reads=C.r_Kd, writes=[r_K[b]])
        P.dma("sp", lambda e: e.dma_start(out=Vh[b][:, :, 0:128], in_=C.Vd[:, h * 128:(h + 1) * 128].rearrange("(i p) d -> p i d", p=128)),
              reads=C.r_Vd, writes=[r_V[b]])
        kw = dict(group=4, regions=tr_regions) if A2_REGIONS else dict()
        yield from transposes_bf16_gen(C, lambda j: Qh[b][:, j, 0:W1], NT, QTn[b], r_QT[b], [r_Q[b]], width=W1, **kw)
        yield from transposes_bf16_gen(C, lambda j: Qh[b][:, j, W1:192], NT, QTr[b], r_QT[b], [r_Q[b]], width=W2, **kw)
        yield from transposes_bf16_gen(C, lambda j: Kh[b][:, j, 0:W1], NT, KTn[b], r_KT[b], [r_K[b]], width=W1, **kw)
        yield from transposes_bf16_gen(C, lambda j: Kh[b][:, j, W1:192], NT, KTr[b], r_KT[b], [r_K[b]], width=W2, **kw)

    def main(h):
        b = h % 2
        qtn = QTn[b][:, :, :].rearrange("p j t -> p (j t)")
        qtr = QTr[b][:, :, :].rearrange("p j t -> p (j t)")
        units = []
        for qg in range(8):
            for kt in range(4 * qg + 4):
                units.append((qg, kt))

        def qk(u):
            qg, kt = units[u]
            j0 = max(0, kt - 4 * qg)
            ncols = (4 - j0) * 128
            q0 = qg * 512 + j0 * 128
            sbk = Sb[u % NS]
            P.op("pe", lambda e: e.matmul(out=sbk[:, 0:ncols], lhsT=KTn[b][:, kt, :], rhs=qtn[:, q0:q0 + ncols], start=True, stop=False),
                 reads=[r_KT[b], r_QT[b]], writes=[r_S[u % NS]], sig=False)
            P.op("pe", lambda e: e.matmul(out=sbk[:, 0:ncols], lhsT=KTr[b][:, kt, :], rhs=qtr[:, q0:q0 + ncols], start=False, stop=True),
                 reads=[r_KT[b], r_QT[b]], writes=[r_S[u % NS]], sig=True)

        def rest(u):
            qg, kt = units[u]
            j0 = max(0, kt - 4 * qg)
            ncols = (4 - j0) * 128
            sbk = Sb[u % NS]
            if kt >= 4 * qg:
                r = dctr[0] % ND
                dctr[0] += 1
                PTu, rPTu = PTd[r], r_PTd[r]
                if ncols > 64:
                    P.op("act", lambda e: e.activation(out=PTu[:, 64:ncols], in_=sbk[:, 64:ncols], func=AF.Exp, scale=scale),
                         reads=[r_S[u % NS]], writes=[rPTu])
                P.op("act", lambda e: e.activation(out=PTu[0:64, 0:64], in_=sbk[0:64, 0:64], func=AF.Exp, scale=scale),
                     reads=[r_S[u % NS]], writes=[rPTu])
            else:
                r = u % NR
                PTu, rPTu = PT[r], r_PT[r]
                P.op("act", lambda e: e.activation(out=PTu[:, 0:ncols], in_=sbk[:, 0:ncols], func=AF.Exp, scale=scale),
                     reads=[r_S[u % NS]], writes=[rPTu])
            for jj in range(j0, 4):
                off = (jj - j0) * 128
                last = (kt == 4 * qg + jj)
                P.op("pe", lambda e: e.matmul(out=acc[jj][:, 0:129], lhsT=PTu[:, off:off + 128], rhs=Vh[b][:, kt, 0:129],
                                              start=(kt == 0), stop=last),
                     reads=[rPTu, r_V[b]], writes=[r_acc[jj]], sig=(last or jj == 3))
                if last:
                    qt = 4 * qg + jj
                    ri = qt % 8
                    P.op("dve", lambda e: e.reciprocal(out=rsum[:, ri:ri + 1], in_=acc[jj][:, 128:129]), reads=[r_acc[jj]], writes=[r_rs[ri]])
                    P.op("dve", lambda e: e.tensor_scalar(out=Oh[b][:, qt, :], in0=acc[jj][:, 0:128], scalar1=rsum[:, ri:ri + 1], scalar2=None, op0=ALU.mult),
                         reads=[r_acc[jj], r_rs[ri]], writes=[r_O[b]])
        qk(0)
        qk(1)
        for u in range(len(units)):
            if u + 2 < len(units):
                qk(u + 2)
            rest(u)
            if u % 18 == 9:
                bg_issue(C, 1, after=[r_S[u % NS]])
            yield
        P.dma("sp", lambda e: e.dma_start(out=C.Od[:, h * 128:(h + 1) * 128].rearrange("(i p) d -> p i d", p=128), in_=Oh[b][:]),
              reads=[r_O[b]], writes=C.r_Od)

    for _ in prologue(0):
        pass
    for h in range(NH):
        interleave(main(h), prologue(h + 1) if h + 1 < NH else None, 8)
    C.bank_pool = None


def phase_proj_moe_front(C, pes, layer):
    nc, P = C.nc, C.P
    load_consts(C, pes)
    if layer == 0:
        A_d, rA, W_d, Xin, rXin, Xout, rXout = C.Od, C.r_Od, C.mla_w_out, C.x, C.r_x, C.x1d, C.r_x1d
    else:
        A_d, rA, W_d, Xin, rXin, Xout, rXout = C.zTd, C.r_zTd, C.pool_w_out, C.x2d, C.r_x2d, C.x3d, C.r_x3d
    wo = sb(pes, nc, "pf_wo", [128, 16, D], BF16)
    r_w = Reg()
    load_w_cast(C, wo, W_d, r_w, 16)
    g_ffn = sb(pes, nc, "pf_g", [128, D], F32)
    wr = sb(pes, nc, "pf_wr", [128, 16, 72], F32)
    bias_b = sb(pes, nc, "pf_bias", [128, 72], F32)
    trif = sb(pes, nc, "pf_trif", [128, 128], F32)
    trib = sb(pes, nc, "pf_trib", [128, 128], BF16)
    onesb = sb(pes, nc, "pf_ones", [128, 128], BF16)
    ebase = sb(pes, nc, "pf_ebase", [128, 64], F32)
    trash_p = sb(pes, nc, "pf_trash", [128, 1], F32)
    trash_i = sb(pes, nc, "pf_trashi", [128, 1], I32)
    mcum = sb(pes, nc, "pf_mcum", [128, 64], F32)
    mcum_b = sb(pes, nc, "pf_mcumb", [128, 64], BF16)
    r_c = Reg()
    r_mc = Reg()
    load_bcast(C, "sp", g_ffn[:], C.ffn_norm[layer:layer + 1, :], D, r_c)
    load_bcast(C, "sp", bias_b[:], C.b_router[layer:layer + 1, :], 72, r_c)
    P.dma("sp", lambda e: e.dma_start(out=wr[:], in_=C.w_router[layer].rearrange("(c p) n -> p c n", p=128)), writes=[r_c])
    P.dma("sp", lambda e: e.dma_start(out=trif[:], in_=C.c_tri), writes=[r_c])
    P.dma("sp", lambda e: e.dma_start(out=ebase[:], in_=C.c_ebase), writes=[r_c])
    P.op("dve", lambda e: e.tensor_copy(out=trib[:], in_=trif[:]), reads=[r_c], writes=[r_c])
    P.op("dve", lambda e: e.memset(onesb[:], 1.0), writes=[r_c])
    P.op("pool", lambda e: e.iota(trash_i[:], pattern=[[0, 1]], base=TRASH, channel_multiplier=1), writes=[r_c])
    P.op("dve", lambda e: e.tensor_copy(out=trash_p[:], in_=trash_i[:]), reads=[r_c], writes=[r_c])
    P.op("dve", lambda e: e.memset(mcum[:], 0.0), writes=[r_mc])
    P.op("dve", lambda e: e.memset(mcum_b[:], 0.0), writes=[r_mc])

    NB = 2
    At = [sb(pes, nc, "pf_A%d" % i, [128, D], BF16) for i in range(NB)]
    Xt = [sb(pes, nc, "pf_X%d" % i, [128, D], F32) for i in range(NB)]
    r_A = [Reg() for _ in range(NB)]
    r_X = [Reg() for _ in range(NB)]
    aT = sb(pes, nc, "pf_aT", [128, 16, 128], BF16)
    r_aT = Reg()
    x1t = [sb(pes, nc, "pf_x1%d" % i, [128, D], F32) for i in range(NB)]
    r_x1 = [Reg() for _ in range(NB)]
    junk = sb(pes, nc, "pf_junk", [128, D], BF16)
    r_junk = Reg()
    hf2 = [sb(pes, nc, "pf_hf%d" % i, [128, D], F32) for i in range(2)]
    r_hf2 = [Reg() for _ in range(2)]
    stA2 = [sb(pes, nc, "pf_stA%d" % i, [128, 2], F32) for i in range(2)]
    r_stA2 = [Reg() for _ in range(2)]
    hb = [sb(pes, nc, "pf_hb%d" % i, [128, D], BF16) for i in range(NB)]
    r_hb = [Reg() for _ in range(NB)]
    hT = sb(pes, nc, "pf_hT", [128, 16, 128], F32)
    r_hT = Reg()
    st = sb(pes, nc, "pf_st", [128, 32], F32)
    r_st = Reg()
    L = sb(pes, nc, "pf_L", [128, 72], F32)
    t8 = sb(pes, nc, "pf_t8", [128, 8, 8], F32)
    t64 = sb(pes, nc, "pf_t64", [128, 8, 8], F32)
    M1 = sb(pes, nc, "pf_M1", [128, 8, 8], F32)
    M2 = sb(pes, nc, "pf_M2", [128, 8, 8], F32)
    Mb = sb(pes, nc, "pf_Mb", [128, 64], BF16)
    sv = sb(pes, nc, "pf_sv", [128, 64], F32)
    r_r = Reg()

    def load(i):
        b = i % NB
        P.dma("sp", lambda e: e.dma_start(out=At[b][:], in_=A_d[i * 128:(i + 1) * 128, :]), reads=[rA[i]], writes=[r_A[b]])
        P.dma("sp", lambda e: e.dma_start(out=Xt[b][:], in_=Xin[i * 128:(i + 1) * 128, :]), reads=[rXin[i]], writes=[r_X[b]])

    def dv(fn, reads=(), writes=()):
        P.op("dve", fn, reads=[r_r] + list(reads), writes=[r_r] + list(writes))

    def stageA(i):
        b = i % NB
        hf, r_hf, stA, r_stA = hf2[b], r_hf2[b], stA2[b], r_stA2[b]
        if i + 1 < NT:
            load(i + 1)
        transposes_bf16(C, lambda j: At[b][:, j * 128:(j + 1) * 128], 16, aT, r_aT, [r_A[b]])
        yield
        for s_ in range(4):
            bank, breg = next_bank(C)
            for c in range(16):
                P.op("pe", lambda e: e.matmul(out=bank[:, :], lhsT=aT[:, c, :], rhs=wo[:, c, s_ * 512:(s_ + 1) * 512], start=(c == 0), stop=(c == 15)),
                     reads=[r_aT, r_w], writes=[breg], sig=(c == 15))
            P.op("dve", lambda e: e.tensor_tensor(out=x1t[b][:, s_ * 512:(s_ + 1) * 512], in0=bank[:, :], in1=Xt[b][:, s_ * 512:(s_ + 1) * 512], op=ALU.add),
                 reads=[breg, r_X[b]], writes=[r_x1[b]])
            yield
        P.dma("sp", lambda e: e.dma_start(out=Xout[i * 128:(i + 1) * 128, :], in_=x1t[b][:]), reads=[r_x1[b]], writes=[rXout[i]])
        P.op("act", lambda e: e.activation(out=junk[:], in_=x1t[b][:], func=AF.Square, accum_out=stA[:, 0:1]), reads=[r_x1[b]], writes=[r_stA])
        rstd_from_ss(C, stA[:, 0:1], stA[:, 1:2], D, r_stA, r_stA, 1)
        P.op("dve", lambda e: e.scalar_tensor_tensor(out=hf[:], in0=x1t[b][:], scalar=stA[:, 1:2], in1=g_ffn[:], op0=ALU.mult, op1=ALU.mult),
             reads=[r_x1[b], r_stA, r_c], writes=[r_hf])
        P.op("act", lambda e: e.activation(out=hb[b][:], in_=hf[:], func=AF.Copy), reads=[r_hf], writes=[r_hb[b]])
        yield

    def stageB(i):
        b = i % NB
        hf, r_hf = hf2[b], r_hf2[b]
        bg_issue(C, 2, after=[r_hf])
        for g0 in range(0, 16, 4):
            bank, breg = next_bank(C)
            for j in range(4):
                c = g0 + j
                P.op("pe", lambda e: e.transpose(out=bank[:, j * 128:(j + 1) * 128], in_=hf[:, c * 128:(c + 1) * 128], identity=C.identf[:]),
                     reads=[r_hf, C.r_ident], writes=[breg], sig=(j == 3))
            if (g0 // 4) % 2 == 0:
                P.op("act", lambda e: e.activation(out=hT[:, g0:g0 + 4, :], in_=bank[:, :].rearrange("p (j t) -> p j t", t=128), func=AF.Copy), reads=[breg], writes=[r_hT])
            else:
                P.op("dve", lambda e: e.tensor_copy(out=hT[:, g0:g0 + 4, :], in_=bank[:, :].rearrange("p (j t) -> p j t", t=128)), reads=[breg], writes=[r_hT])
        yield
        lbank, lreg = next_bank(C)
        for c in range(16):
            P.op("pe", lambda e: e.matmul(out=lbank[:, 0:72], lhsT=hT[:, c, :], rhs=wr[:, c, :], start=(c == 0), stop=(c == 15)),
                 reads=[r_hT, r_c], writes=[lreg], sig=(c == 15))
        yield
        G = L[:, 0:8]
        LE = L[:, 8:72].rearrange("p (g e) -> p g e", g=8)
        gm, ngm, sg, psel = st[:, 2:3], st[:, 3:4], st[:, 4:5], st[:, 5:6]
        m1, m2, dd, ee, den, qv1, qv2 = st[:, 6:7], st[:, 7:8], st[:, 8:9], st[:, 9:10], st[:, 10:11], st[:, 11:12], st[:, 12:13]
        s1, s2, k1, k2 = st[:, 13:14], st[:, 14:15], st[:, 15:16], st[:, 16:17]
        goh = t8[:, 0, :]
        ex8 = t8[:, 1, :]
        les = t8[:, 2, :]
        oh1 = t8[:, 3, :]
        les2 = t8[:, 4, :]
        oh2 = t8[:, 5, :]
        dv(lambda e: e.tensor_tensor(out=L[:, :], in0=lbank[:, 0:72], in1=bias_b[:, :], op=ALU.add), reads=[lreg, r_c])
        dv(lambda e: e.tensor_reduce(out=gm, in_=G, axis=AX.X, op=ALU.max))
        dv(lambda e: e.tensor_scalar(out=goh, in0=G, scalar1=gm, scalar2=None, op0=ALU.is_equal))
        dv(lambda e: e.tensor_scalar(out=ngm, in0=gm, scalar1=-1.0, scalar2=None, op0=ALU.mult))
        P.op("act", lambda e: e.activation(out=ex8, in_=G, func=AF.Exp, bias=ngm, scale=1.0, accum_out=sg), reads=[r_r], writes=[r_r])
        dv(lambda e: e.reciprocal(out=psel, in_=sg))
        yield
        dv(lambda e: e.tensor_tensor(out=t64[:, :, :], in0=LE, in1=goh.unsqueeze(2).broadcast_to([128, 8, 8]), op=ALU.mult))
        dv(lambda e: e.tensor_reduce(out=les, in_=t64[:, :, :].rearrange("p g e -> p e g"), axis=AX.X, op=ALU.add))
        dv(lambda e: e.tensor_reduce(out=m1, in_=les, axis=AX.X, op=ALU.max))
        dv(lambda e: e.tensor_scalar(out=oh1, in0=les, scalar1=m1, scalar2=None, op0=ALU.is_equal))
        dv(lambda e: e.scalar_tensor_tensor(out=les2, in0=oh1, scalar=-1e30, in1=les, op0=ALU.mult, op1=ALU.add))
        dv(lambda e: e.tensor_reduce(out=m2, in_=les2, axis=AX.X, op=ALU.max))
        dv(lambda e: e.tensor_scalar(out=oh2, in0=les2, scalar1=m2, scalar2=None, op0=ALU.is_equal))
        yield
        dv(lambda e: e.tensor_tensor(out=dd, in0=m2, in1=m1, op=ALU.subtract))
        P.op("act", lambda e: e.activation(out=ee, in_=dd, func=AF.Exp), reads=[r_r], writes=[r_r])
        dv(lambda e: e.tensor_scalar(out=den, in0=ee, scalar1=1.0, scalar2=None, op0=ALU.add))
        dv(lambda e: e.reciprocal(out=qv1, in_=den))
        dv(lambda e: e.tensor_tensor(out=qv2, in0=ee, in1=qv1, op=ALU.mult))
        dv(lambda e: e.tensor_tensor(out=qv1, in0=qv1, in1=psel, op=ALU.mult))
        dv(lambda e: e.tensor_tensor(out=qv2, in0=qv2, in1=psel, op=ALU.mult))
        yield
        dv(lambda e: e.tensor_tensor(out=M1[:, :, :], in0=goh.unsqueeze(2).broadcast_to([128, 8, 8]), in1=oh1.unsqueeze(1).broadcast_to([128, 8, 8]), op=ALU.mult))
        dv(lambda e: e.tensor_tensor(out=M2[:, :, :], in0=goh.unsqueeze(2).broadcast_to([128, 8, 8]), in1=oh2.unsqueeze(1).broadcast_to([128, 8, 8]), op=ALU.mult))
        M1f = M1[:, :, :].rearrange("p g e -> p (g e)")
        M2f = M2[:, :, :].rearrange("p g e -> p (g e)")
        t64f = t64[:, :, :].rearrange("p g e -> p (g e)")
        dv(lambda e: e.tensor_tensor(out=t64f, in0=M1f, in1=M2f, op=ALU.add))
        dv(lambda e: e.tensor_copy(out=Mb[:, :], in_=t64f))
        pbank, preg = next_bank(C)
        P.op("pe", lambda e: e.matmul(out=pbank[:, 0:64], lhsT=trib[:, :], rhs=Mb[:, :], start=True, stop=False), reads=[r_r, r_c], writes=[preg], sig=False)
        P.op("pe", lambda e: e.matmul(out=pbank[:, 0:64], lhsT=onesb[:, :], rhs=mcum_b[:, :], start=False, stop=True), reads=[r_r, r_c, r_mc], writes=[preg], sig=True)
        dv(lambda e: e.tensor_tensor(out=sv[:, :], in0=pbank[:, 0:64], in1=ebase[:, :], op=ALU.add), reads=[preg, r_c])
        yield
        ovf = L[:, 0:64]
        dv(lambda e: e.tensor_single_scalar(out=ovf, in_=pbank[:, 0:64], scalar=CAP - 0.5, op=ALU.is_gt), reads=[preg])
        dv(lambda e: e.tensor_scalar(out=t64f, in0=ovf, scalar1=-1.0, scalar2=1.0, op0=ALU.mult, op1=ALU.add))
        dv(lambda e: e.tensor_tensor(out=sv[:, :], in0=sv[:, :], in1=t64f, op=ALU.mult))
        dv(lambda e: e.scalar_tensor_tensor(out=sv[:, :], in0=ovf, scalar=trash_p[:, 0:1], in1=sv[:, :], op0=ALU.mult, op1=ALU.add), reads=[r_c])
        dv(lambda e: e.tensor_tensor(out=t64f, in0=M1f, in1=M2f, op=ALU.add))
        P.op("dve", lambda e: e.tensor_tensor(out=mcum[:, :], in0=mcum[:, :], in1=t64f, op=ALU.add), reads=[r_r, r_mc], writes=[r_mc])
        P.op("dve", lambda e: e.tensor_copy(out=mcum_b[:, :], in_=mcum[:, :]), reads=[r_mc], writes=[r_mc])
        yield
        dv(lambda e: e.tensor_tensor(out=t64f, in0=M1f, in1=sv[:, :], op=ALU.mult))
        dv(lambda e: e.tensor_reduce(out=s1, in_=t64f, axis=AX.X, op=ALU.add))
        dv(lambda e: e.tensor_tensor(out=t64f, in0=M2f, in1=sv[:, :], op=ALU.mult))
        dv(lambda e: e.tensor_reduce(out=s2, in_=t64f, axis=AX.X, op=ALU.add))
        dv(lambda e: e.tensor_single_scalar(out=k1, in_=s1, scalar=TRASH - 0.5, op=ALU.is_lt))
        dv(lambda e: e.tensor_single_scalar(out=k2, in_=s2, scalar=TRASH - 0.5, op=ALU.is_lt))
        rs_ = C.r_slot[i]
        dv(lambda e: e.tensor_tensor(out=C.gate_w[:, i, 0:1], in0=qv1, in1=k1, op=ALU.mult), writes=[rs_])
        dv(lambda e: e.tensor_tensor(out=C.gate_w[:, i, 1:2], in0=qv2, in1=k2, op=ALU.mult), writes=[rs_])
        dv(lambda e: e.tensor_copy(out=C.slot_i[:, i, 0:1], in_=s1), writes=[rs_])
        dv(lambda e: e.tensor_copy(out=C.slot_i[:, i, 1:2], in_=s2), writes=[rs_])
        for k in range(2):
            P.dma("pool", lambda e: e.indirect_dma_start(out=C.xgd, out_offset=bass.IndirectOffsetOnAxis(ap=C.slot_i[:, i, k:k + 1], axis=0),
                                                         in_=hb[b][:, :], in_offset=None),
                  reads=[r_hb[b], rs_], writes=[C.r_xgt[i]])

    load(0)
    for _ in stageA(0):
        pass
    for i in range(NT):
        interleave(stageB(i), stageA(i + 1) if i + 1 < NT else None, 1)
    if "dbg" in C.debug_out:
        sf = sb(pes, nc, "pf_sf", [128, NT, 2], F32)
        P.op("dve", lambda e: e.tensor_copy(out=sf[:], in_=C.slot_i[:]), reads=C.r_slot, writes=[r_r])
        dview = C.dbg.rearrange("(i p) c -> p i c", p=128)
        P.dma("sp", lambda e: e.dma_start(out=dview[:, :, 0:2], in_=C.gate_w[:]), reads=C.r_slot)
        P.dma("sp", lambda e: e.dma_start(out=dview[:, :, 2:4], in_=sf[:]), reads=[r_r])


def phase_experts(C, pes, layer):
    nc, P = C.nc, C.P
    load_consts(C, pes)
    NB = 2
    NW = 3
    wg = [sb(pes, nc, "ex_wg%d" % i, [128, 16, 512], BF16) for i in range(NW)]
    wu = [sb(pes, nc, "ex_wu%d" % i, [128, 16, 512], BF16) for i in range(NW)]
    wd = [sb(pes, nc, "ex_wd%d" % i, [128, 4, D], BF16) for i in range(NW)]
    r_wg = [Reg() for _ in range(NW)]
    r_wu = [Reg() for _ in range(NW)]
    r_wd = [Reg() for _ in range(NW)]
    Xe = [sb(pes, nc, "ex_X%d" % i, [128, 2, D], BF16) for i in range(NB)]
    r_Xe = [Reg() for _ in range(NB)]
    xT2 = [sb(pes, nc, "ex_xT%d" % i, [128, 16, 256], BF16) for i in range(2)]
    r_xT2 = [Reg() for _ in range(2)]
    sg = [sb(pes, nc, "ex_sg%d" % i, [128, 256], F32) for i in range(2)]
    r_sg = [Reg() for _ in range(2)]
    hT2 = [sb(pes, nc, "ex_hT%d" % i, [128, 4, 256], BF16) for i in range(2)]
    r_hT2 = [Reg() for _ in range(2)]
    Ye = [sb(pes, nc, "ex_Y%d" % i, [128, 2, D], BF16) for i in range(NB)]
    r_Ye = [Reg() for _ in range(NB)]
    zt = sb(pes, nc, "ex_zero", [128, D], BF16)
    r_z = Reg()
    P.op("dve", lambda e: e.memset(zt[:], 0.0), writes=[r_z])
    P.dma("sp", lambda e: e.dma_start(out=C.ygd[NSLOT:NSLOT + 128, :], in_=zt[:]), reads=[r_z], writes=[C.r_ygt])

    nconv = NCONV0 if layer == 0 else NCONV1
    order = list(range(nconv, 64)) + list(range(nconv))
    while C.bg_items and C.bg_items[0][0] == layer:
        bg_issue(C, 1)

    def loadw(k):
        ex = order[k]
        b = k % NW
        if ex < nconv:
            P.dma("sp", lambda e: e.dma_start(out=wg[b][:], in_=C.wgb[layer, ex].rearrange("p (c n) -> p c n", c=16)),
                  reads=[C.r_wb[(layer, ex, 0)]], writes=[r_wg[b]])
            P.dma("sp", lambda e: e.dma_start(out=wu[b][:], in_=C.wub[layer, ex].rearrange("p (c n) -> p c n", c=16)),
                  reads=[C.r_wb[(layer, ex, 1)]], writes=[r_wu[b]])
            P.dma("sp", lambda e: e.dma_start(out=wd[b][:], in_=C.wdb[layer, ex].rearrange("p (c n) -> p c n", c=4)),
                  reads=[C.r_wb[(layer, ex, 2)]], writes=[r_wd[b]])
            return
        for c0 in (0, 8):
            P.dma("pool", lambda e: e.dma_start(out=wg[b][:, c0:c0 + 8, :], in_=C.w_gate[layer, ex, c0 * 128:(c0 + 8) * 128, :].rearrange("(c p) n -> p c n", p=128)),
                  writes=[r_wg[b]])
        for c0 in (0, 8):
            P.dma("pool", lambda e: e.dma_start(out=wu[b][:, c0:c0 + 8, :], in_=C.w_up[layer, ex, c0 * 128:(c0 + 8) * 128, :].rearrange("(c p) n -> p c n", p=128)),
                  writes=[r_wu[b]])
        for c0 in (0, 2):
            P.dma("pool", lambda e: e.dma_start(out=wd[b][:, c0:c0 + 2, :], in_=C.w_down[layer, ex, c0 * 128:(c0 + 2) * 128, :].rearrange("(c p) n -> p c n", p=128)),
                  writes=[r_wd[b]])

    def loadx(k):
        ex = order[k]
        b = k % NB
        P.dma("sp", lambda e: e.dma_start(out=Xe[b][:], in_=C.xgd[ex * CAP:(ex + 1) * CAP, :].rearrange("(t p) d -> p t d", p=128)),
              reads=C.r_xgt, writes=[r_Xe[b]])

    loadw(0)
    loadw(1)
    loadx(0)
    for k in range(64):
        ex = order[k]
        b = k % NB
        w = k % NW
        if k + 2 < 64:
            loadw(k + 2)
        if k + 1 < 64:
            loadx(k + 1)
        xT, r_xT, hT, r_hT = xT2[b], r_xT2[b], hT2[b], r_hT2[b]
        transposes_bf16(C, lambda j: Xe[b][:, j % 2, (j // 2) * 128:(j // 2 + 1) * 128], 32,
                        xT[:, :, :].rearrange("p c (t k) -> p (c t) k", t=2), r_xT, [r_Xe[b]])
        for fc in range(4):
            bank, breg = next_bank(C)
            for c in range(16):
                P.op("pe", lambda e: e.matmul(out=bank[:, 0:256], lhsT=wg[w][:, c, fc * 128:(fc + 1) * 128], rhs=xT[:, c, :], start=(c == 0), stop=(c == 15)),
                     reads=[r_xT, r_wg[w]], writes=[breg], sig=(c == 15))
            for c in range(16):
                P.op("pe", lambda e: e.matmul(out=bank[:, 256:512], lhsT=wu[w][:, c, fc * 128:(fc + 1) * 128], rhs=xT[:, c, :], start=(c == 0), stop=(c == 15)),
                     reads=[r_xT, r_wu[w]], writes=[breg], sig=(c == 15))
            si = fc % 2
            P.op("act", lambda e: e.activation(out=sg[si][:, :], in_=bank[:, 0:256], func=AF.Silu), reads=[breg], writes=[r_sg[si]])
            P.op("dve", lambda e: e.tensor_tensor(out=hT[:, fc, :], in0=bank[:, 256:512], in1=sg[si][:, :], op=ALU.mult), reads=[breg, r_sg[si]], writes=[r_hT])
        for t in range(2):
            for sl in range(4):
                bank, breg = next_bank(C)
                for fc in range(4):
                    P.op("pe", lambda e: e.matmul(out=bank[:, :], lhsT=hT[:, fc, t * 128:(t + 1) * 128], rhs=wd[w][:, fc, sl * 512:(sl + 1) * 512], start=(fc == 0), stop=(fc == 3)),
                         reads=[r_hT, r_wd[w]], writes=[breg], sig=(fc == 3))
                if sl % 2 == 0:
                    P.op("act", lambda e: e.activation(out=Ye[b][:, t, sl * 512:(sl + 1) * 512], in_=bank[:, :], func=AF.Copy), reads=[breg], writes=[r_Ye[b]])
                else:
                    P.op("dve", lambda e: e.tensor_copy(out=Ye[b][:, t, sl * 512:(sl + 1) * 512], in_=bank[:, :]), reads=[breg], writes=[r_Ye[b]])
        P.dma("sp", lambda e: e.dma_start(out=C.ygd[ex * CAP:(ex + 1) * CAP, :].rearrange("(t p) d -> p t d", p=128), in_=Ye[b][:]),
              reads=[r_Ye[b]], writes=[C.r_yg[ex]])


def phase_combine(C, pes, layer):
    nc, P = C.nc, C.P
    if layer == 0:
        Xin, rXin, Xout, rXout = C.x1d, C.r_x1d, C.x2d, C.r_x2d
    else:
        Xin, rXin, Xout, rXout = C.x3d, C.r_x3d, C.out, C.r_out
    NB = 2
    Y1 = [sb(pes, nc, "cb_Y1%d" % i, [128, D], BF16) for i in range(NB)]
    Y2 = [sb(pes, nc, "cb_Y2%d" % i, [128, D], BF16) for i in range(NB)]
    Xt = [sb(pes, nc, "cb_X%d" % i, [128, D], F32) for i in range(NB)]
    Ot = [sb(pes, nc, "cb_O%d" % i, [128, D], F32) for i in range(NB)]
    r_Y1 = [Reg() for _ in range(NB)]
    r_Y2 = [Reg() for _ in range(NB)]
    r_X = [Reg() for _ in range(NB)]
    r_O = [Reg() for _ in range(NB)]
    yregs = list(C.r_yg) + [C.r_ygt]

    def load(i):
        b = i % NB
        P.dma("pool", lambda e: e.indirect_dma_start(out=Y1[b][:, :], out_offset=None, in_=C.ygd,
                                                     in_offset=bass.IndirectOffsetOnAxis(ap=C.slot_i[:, i, 0:1], axis=0)),
              reads=yregs + [C.r_slot[i]], writes=[r_Y1[b]])
        P.dma("pool", lambda e: e.indirect_dma_start(out=Y2[b][:, :], out_offset=None, in_=C.ygd,
                                                     in_offset=bass.IndirectOffsetOnAxis(ap=C.slot_i[:, i, 1:2], axis=0)),
              reads=yregs + [C.r_slot[i]], writes=[r_Y2[b]])
        P.dma("sp", lambda e: e.dma_start(out=Xt[b][:], in_=Xin[i * 128:(i + 1) * 128, :]), reads=[rXin[i]], writes=[r_X[b]])

    load(0)
    for i in range(NT):
        b = i % NB
        if i + 1 < NT:
            load(i + 1)
        P.op("dve", lambda e: e.scalar_tensor_tensor(out=Ot[b][:], in0=Y1[b][:], scalar=C.gate_w[:, i, 0:1], in1=Xt[b][:], op0=ALU.mult, op1=ALU.add),
             reads=[r_Y1[b], r_X[b], C.r_slot[i]], writes=[r_O[b]])
        P.op("dve", lambda e: e.scalar_tensor_tensor(out=Ot[b][:], in0=Y2[b][:], scalar=C.gate_w[:, i, 1:2], in1=Ot[b][:], op0=ALU.mult, op1=ALU.add),
             reads=[r_Y2[b], C.r_slot[i]], writes=[r_O[b]])
        P.dma("sp", lambda e: e.dma_start(out=Xout[i * 128:(i + 1) * 128, :], in_=Ot[b][:]), reads=[r_O[b]], writes=[rXout[i]])


def phase_pool_a(C, pes):
    nc, P = C.nc, C.P
    load_consts(C, pes)
    w_in = sb(pes, nc, "pl_win", [128, 16, D], BF16)
    wgrp = sb(pes, nc, "pl_wg", [128, 16, 512], BF16)
    r_w = Reg()
    load_w_cast(C, w_in, C.pool_w_in, r_w, 16)
    for g in range(4):
        P.dma("pool", lambda e: e.dma_start(out=wgrp[:, g * 4:(g + 1) * 4, :], in_=C.pool_w_group[g].rearrange("(c p) n -> p c n", p=128)), writes=[r_w])
    g_mix = sb(pes, nc, "pl_g", [128, D], F32)
    scale_b = sb(pes, nc, "pl_scale", [128, D], F32)
    bandf = sb(pes, nc, "pl_bandf", [128, 12, 128], F32)
    bandb = sb(pes, nc, "pl_bandb", [128, 12, 128], BF16)
    r_c = Reg()
    load_bcast(C, "sp", g_mix[:], C.mix_norm[1:2, :], D, r_c)
    load_bcast(C, "sp", scale_b[:], C.pool_scale[0:1, :], D, r_c)
    P.dma("sp", lambda e: e.dma_start(out=bandf[:], in_=C.c_band.rearrange("g k a b -> a (g k) b")), writes=[r_c])
    P.op("dve", lambda e: e.tensor_copy(out=bandb[:], in_=bandf[:]), reads=[r_c], writes=[r_c])
    NB = 2
    xt = [sb(pes, nc, "pl_xt%d" % i, [128, D], F32) for i in range(NB)]
    r_xt = [Reg() for _ in range(NB)]
    junk = sb(pes, nc, "pl_junk", [128, D], BF16)
    r_junk = Reg()
    st = sb(pes, nc, "pl_st", [128, 4], F32)
    r_st = Reg()
    hb = sb(pes, nc, "pl_hb", [128, D], BF16)
    r_hb = Reg()
    hT = sb(pes, nc, "pl_hT", [128, 16, 128], BF16)
    r_hT = Reg()
    st2 = [sb(pes, nc, "pl_st%d" % i, [128, 4], F32) for i in range(2)]
    r_st2 = [Reg() for _ in range(2)]
    u = [sb(pes, nc, "pl_u%d" % i, [128, D], BF16) for i in range(3)]
    r_u = [Reg() for _ in range(3)]
    pT = sb(pes, nc, "pl_pT", [128, 16, 128], BF16)
    r_pT = Reg()
    zt = [sb(pes, nc, "pl_z%d" % i, [128, D], BF16) for i in range(NB)]
    r_z = [Reg() for _ in range(NB)]

    def load(i):
        b = i % NB
        P.dma("sp", lambda e: e.dma_start(out=xt[b][:], in_=C.x2d[i * 128:(i + 1) * 128, :]), reads=[C.r_x2d[i]], writes=[r_xt[b]])

    def stageA(i):
        b = i % NB
        if i + 1 < NT:
            load(i + 1)
        X = xt[b]
        st, r_st = st2[b], r_st2[b]
        P.op("act", lambda e: e.activation(out=junk[:], in_=X[:], func=AF.Square, accum_out=st[:, 0:1]), reads=[r_xt[b]], writes=[r_st])
        rstd_from_ss(C, st[:, 0:1], st[:, 1:2], D, r_st, r_st, 1)
        P.op("dve", lambda e: e.scalar_tensor_tensor(out=hb[:], in0=X[:], scalar=st[:, 1:2], in1=g_mix[:], op0=ALU.mult, op1=ALU.mult),
             reads=[r_xt[b], r_st, r_c], writes=[r_hb])
        yield
        transposes_bf16(C, lambda j: hb[:, j * 128:(j + 1) * 128], 16, hT, r_hT, [r_hb])
        yield
        U, Up = u[i % 3], u[(i + 2) % 3]
        rU, rUp = r_u[i % 3], r_u[(i + 2) % 3]
        for s_ in range(4):
            bank, breg = next_bank(C)
            for c in range(16):
                P.op("pe", lambda e: e.matmul(out=bank[:, :], lhsT=hT[:, c, :], rhs=w_in[:, c, s_ * 512:(s_ + 1) * 512], start=(c == 0), stop=(c == 15)),
                     reads=[r_hT, r_w], writes=[breg], sig=(c == 15))
            if s_ % 2 == 0:
                P.op("act", lambda e: e.activation(out=U[:, s_ * 512:(s_ + 1) * 512], in_=bank[:, :], func=AF.Copy), reads=[breg], writes=[rU])
            else:
                P.op("dve", lambda e: e.tensor_copy(out=U[:, s_ * 512:(s_ + 1) * 512], in_=bank[:, :]), reads=[breg], writes=[rU])
            yield

    def stageB(i):
        b = i % NB
        U, Up = u[i % 3], u[(i + 2) % 3]
        rU, rUp = r_u[i % 3], r_u[(i + 2) % 3]
        bg_issue(C, 1 + (i % 2), after=[rU])
        for g0 in range(0, 16, 4):
            bank, breg = next_bank(C)
            for j in range(4):
                c = g0 + j
                gi = c // 4
                kd = 0 if i == 0 else 1
                P.op("pe", lambda e: e.matmul(out=bank[:, j * 128:(j + 1) * 128], lhsT=U[:, c * 128:(c + 1) * 128], rhs=bandb[:, gi * 3 + kd, :], start=True, stop=(i == 0)),
                     reads=[rU, r_c], writes=[breg], sig=(i == 0 and j == 3))
                if i > 0:
                    P.op("pe", lambda e: e.matmul(out=bank[:, j * 128:(j + 1) * 128], lhsT=Up[:, c * 128:(c + 1) * 128], rhs=bandb[:, gi * 3 + 2, :], start=False, stop=True),
                         reads=[rUp, r_c], writes=[breg], sig=(j == 3))
            if (g0 // 4) % 2 == 0:
                P.op("act", lambda e: e.activation(out=pT[:, g0:g0 + 4, :], in_=bank[:, :].rearrange("p (j t) -> p j t", t=128), func=AF.Copy), reads=[breg], writes=[r_pT])
            else:
                P.op("dve", lambda e: e.tensor_copy(out=pT[:, g0:g0 + 4, :], in_=bank[:, :].rearrange("p (j t) -> p j t", t=128)), reads=[breg], writes=[r_pT])
            yield
        for g in range(4):
            bank, breg = next_bank(C)
            for cc in range(4):
                P.op("pe", lambda e: e.matmul(out=bank[:, :], lhsT=pT[:, g * 4 + cc, :], rhs=wgrp[:, g * 4 + cc, :], start=(cc == 0), stop=(cc == 3)),
                     reads=[r_pT, r_w], writes=[breg], sig=(cc == 3))
            P.op("dve", lambda e: e.tensor_tensor(out=zt[b][:, g * 512:(g + 1) * 512], in0=bank[:, :], in1=scale_b[:, g * 512:(g + 1) * 512], op=ALU.mult),
                 reads=[breg, r_c], writes=[r_z[b]])
            yield
        P.dma("sp", lambda e: e.dma_start(out=C.zTd[i * 128:(i + 1) * 128, :], in_=zt[b][:]), reads=[r_z[b]], writes=[C.r_zTd[i]])

    load(0)
    for _ in stageA(0):
        pass
    for i in range(NT):
        interleave(stageB(i), stageA(i + 1) if i + 1 < NT else None, 1)


def host_constants():
    c = {}
    c["c_ident"] = np.eye(128, dtype=np.float32)
    t = np.arange(128)
    c["c_tri"] = (t[:, None] < t[None, :]).astype(np.float32)
    invf = (1.0 / (10000.0 ** (np.arange(0, 64, 2, dtype=np.float32) / 64))).astype(np.float32)
    c["c_invf"] = np.broadcast_to(invf[None, :], (128, 32)).copy()
    band = np.zeros((4, 3, 128, 128), np.float32)
    for gi, w in enumerate((2, 4, 8, 16)):
        for kind in range(3):
            for tt in range(128):
                for j in range(w):
                    src = tt - j
                    if kind == 0:
                        if src >= 0:
                            band[gi, 0, src, tt] += 1.0 / min(tt + 1, w)
                    elif kind == 1:
                        if src >= 0:
                            band[gi, 1, src, tt] += 1.0 / w
                    else:
                        if src < 0:
                            band[gi, 2, 128 + src, tt] += 1.0 / w
            if kind < 2:
                pass
        band[gi, 0] -= np.eye(128, dtype=np.float32)
        band[gi, 1] -= np.eye(128, dtype=np.float32)
    c["c_band"] = band
    c["c_ebase"] = np.broadcast_to((np.arange(64, dtype=np.float32) * CAP)[None, :], (128, 64)).copy()
    return c


def core_inputs(inp, b, consts, with_moe=True):
    m = {}
    m["x"] = np.ascontiguousarray(inp["x"][b])
    m["pos"] = np.ascontiguousarray(np.asarray(inp["positions"][b]).reshape(NT, 128).T)
    m["mix_norm"] = inp["mix_norm"]
    m["mla_w_in"] = inp["mla_w_in"][0]
    m["mla_q_lat_norm"] = inp["mla_q_lat_norm"]
    m["mla_kv_lat_norm"] = inp["mla_kv_lat_norm"]
    m["mla_w_q_up"] = inp["mla_w_q_up"][0]
    m["mla_w_kv_up"] = inp["mla_w_kv_up"][0]
    m["mla_q_norm"] = inp["mla_q_norm"]
    m["mla_k_norm"] = inp["mla_k_norm"]
    m["mla_w_out"] = inp["mla_w_out"][0]
    m["pool_w_in"] = inp["pool_w_in"][0]
    m["pool_w_group"] = inp["pool_w_group"][0]
    m["pool_scale"] = inp["pool_scale"]
    m["pool_w_out"] = inp["pool_w_out"][0]
    m["ffn_norm"] = inp["ffn_norm"]
    m["w_router"] = np.ascontiguousarray(np.concatenate([inp["moe_w_router_group"], inp["moe_w_router_expert"]], axis=2))
    m["b_router"] = np.ascontiguousarray(np.concatenate([inp["moe_b_router_group"], np.asarray(inp["moe_b_router_expert"]).reshape(2, 64)], axis=1))
    if with_moe:
        m["moe_w_gate"] = inp["moe_w_gate"]
        m["moe_w_up"] = inp["moe_w_up"]
        m["moe_w_down"] = inp["moe_w_down"]
    m.update(consts)
    return {k: np.ascontiguousarray(np.asarray(v)) for k, v in m.items()}


_CACHE = {}


def kernel(**inputs):
    inp = {k: np.asarray(v) for k, v in inputs.items()}
    consts = host_constants()
    if "nc" not in _CACHE:
        _CACHE["nc"] = build_program()
    nc = _CACHE["nc"]
    in_maps = [core_inputs(inp, b, consts) for b in range(8)]
    res = run_bass_kernel_spmd(nc, in_maps, core_ids=list(range(8)))
    return np.stack([np.asarray(res.results[b]["out"]) for b in range(8)], axis=0).astype(np.float32)
```

```python
import contextlib
import numpy as np
import ml_dtypes
import concourse.bass as bass
import concourse.mybir as mybir
from concourse.bass_utils import run_bass_kernel_spmd

F32 = mybir.dt.float32
BF16 = mybir.dt.bfloat16
I32 = mybir.dt.int32
AF = mybir.ActivationFunctionType
ALU = mybir.AluOpType
AX = mybir.AxisListType

NDMA_SEMS = 24


class Reg:
    __slots__ = ("w", "r", "name")

    def __init__(self, name=""):
        self.w = None
        self.r = {}
        self.name = name


class _Cap:
    def __getattr__(self, name):
        return lambda *a, **k: (name, a, k)


_CAP = _Cap()


class Rec:
    ENGS = ("pe", "act", "dve", "pool", "sp")

    def __init__(self, nc, es):
        self.nc = nc
        self.sems = {}
        self.count = {}
        for e in self.ENGS:
            self.sems[e] = es.enter_context(nc.semaphore("s_" + e))
            self.count[e] = 0
        for i in range(NDMA_SEMS):
            k = "d%d" % i
            self.sems[k] = es.enter_context(nc.semaphore("s_" + k))
            self.count[k] = 0
        self.dma_rr = 0
        self.ops = {e: [] for e in self.ENGS}
        self.seen = {e: {} for e in self.ENGS}

    def _deps(self, eng, reads, writes):
        deps = {}

        def add(tok):
            if tok is None:
                return
            k, v = tok
            if deps.get(k, 0) < v:
                deps[k] = v
        for R in reads:
            add(R.w)
        for W in writes:
            if W.w is not None and not (W.w[0] == eng and eng in ("act", "dve")):
                add(W.w)
            for k, v in W.r.items():
                add((k, v))
        if eng == "pe":
            deps.pop("pe", None)
        return deps

    def _waits(self, eng, deps):
        seen = self.seen[eng]
        w = []
        for k, v in deps.items():
            if seen.get(k, 0) < v:
                seen[k] = v
                w.append((k, v))
        return w

    def _commit(self, tok, reads, writes):
        k, v = tok
        for R in reads:
            if R.r.get(k, 0) < v:
                R.r[k] = v
        for W in writes:
            W.w = tok
            W.r = {}

    def op(self, eng, fn, reads=(), writes=(), sig=True):
        deps = self._deps(eng, reads, writes)
        waits = self._waits(eng, deps)
        inc = None
        if sig:
            self.count[eng] += 1
            tok = (eng, self.count[eng])
            inc = (eng, 1)
            self._commit(tok, reads, writes)
        self.ops[eng].append((waits, fn(_CAP), inc))

    def dma(self, q, fn, reads=(), writes=(), after=()):
        deps = self._deps(q, reads, writes)
        for R in after:
            if R.w is not None and deps.get(R.w[0], 0) < R.w[1]:
                deps[R.w[0]] = R.w[1]
        k = "d%d" % self.dma_rr
        self.dma_rr = (self.dma_rr + 1) % NDMA_SEMS
        if self.count[k] > 0:
            if deps.get(k, 0) < self.count[k]:
                deps[k] = self.count[k]
        waits = self._waits(q, deps)
        self.count[k] += 16
        tok = (k, self.count[k])
        self._commit(tok, reads, writes)
        self.ops[q].append((waits, fn(_CAP), (k, 16)))

    def drain(self):
        deps = {}
        for i in range(NDMA_SEMS):
            k = "d%d" % i
            if self.count[k] > 0:
                deps[k] = self.count[k]
        waits = self._waits("sp", deps)
        if waits:
            self.ops["sp"].append((waits, None, None))

    def emit(self, name=None):
        nc = self.nc
        ops = self.ops
        self.ops = {e: [] for e in self.ENGS}
        sems = self.sems

        def replay(e, lst):
            for waits, fn, inc in lst:
                for k, v in waits:
                    e.wait_ge(sems[k], v)
                if fn is None:
                    continue
                name, a, kw = fn
                ins = getattr(e, name)(*a, **kw)
                if inc is not None:
                    ins.then_inc(sems[inc[0]], inc[1])

        with nc.Block(name) as block:
            @block.tensor
            def _(e):
                replay(e, ops["pe"])

            @block.scalar
            def _(e):
                replay(e, ops["act"])

            @block.vector
            def _(e):
                replay(e, ops["dve"])

            @block.gpsimd
            def _(e):
                replay(e, ops["pool"])

            @block.sync
            def _(e):
                replay(e, ops["sp"])


S = 4096
D = 2048
NT = S // 128
NH = 16
CAP = 256
NSLOT = 64 * CAP
TRASH = NSLOT
EPS = 1e-6
TWO_PI = 6.283185307179586
PI = 3.141592653589793


NCONV0 = 64
NCONV1 = 37


class Ctx:
    pass


def bg_issue(C, n, after=()):
    for _ in range(n):
        if not C.bg_items:
            return
        layer, ex, which = C.bg_items.pop(0)
        reg = C.r_wb[(layer, ex, which)]
        C.bg_hist.append(reg)
        deps = list(after)
        if len(C.bg_hist) > 4:
            deps.append(C.bg_hist[-5])
        if which == 0:
            src, dst, kc = C.w_gate[layer, ex], C.wgb[layer, ex], 16
        elif which == 1:
            src, dst, kc = C.w_up[layer, ex], C.wub[layer, ex], 16
        else:
            src, dst, kc = C.w_down[layer, ex], C.wdb[layer, ex], 4
        C.P.dma("pool", lambda e: e.dma_start(out=dst.rearrange("p (c n) -> p c n", c=kc),
                                              in_=src.rearrange("(c p) n -> p c n", p=128)), after=deps, writes=[reg])


def _dram(nc, name, shape, dt, kind):
    return nc.dram_tensor(name, list(shape), dt, kind=kind).ap()


def build_program(debug_out=(), phases=("a1", "a2", "a3", "m2_0", "m3_0", "l1a", "l1b", "m2_1", "m3_1")):
    nc = bass.Bass("TRN2", target_bir_lowering=False)
    C = Ctx()
    C.nc = nc
    C.debug_out = tuple(debug_out)
    I = lambda n, s, d: _dram(nc, n, s, d, "ExternalInput")
    C.x = I("x", [S, D], F32)
    C.pos = I("pos", [128, NT], I32)
    C.mix_norm = I("mix_norm", [2, D], F32)
    C.mla_w_in = I("mla_w_in", [D, 1088], F32)
    C.q_lat = I("mla_q_lat_norm", [1, 512], F32)
    C.kv_lat = I("mla_kv_lat_norm", [1, 512], F32)
    C.w_q_up = I("mla_w_q_up", [512, 3072], F32)
    C.w_kv_up = I("mla_w_kv_up", [512, 4096], F32)
    C.q_norm = I("mla_q_norm", [1, 192], F32)
    C.k_norm = I("mla_k_norm", [1, 192], F32)
    C.mla_w_out = I("mla_w_out", [D, D], F32)
    C.pool_w_in = I("pool_w_in", [D, D], F32)
    C.pool_w_group = I("pool_w_group", [4, 512, 512], F32)
    C.pool_scale = I("pool_scale", [1, D], F32)
    C.pool_w_out = I("pool_w_out", [D, D], F32)
    C.ffn_norm = I("ffn_norm", [2, D], F32)
    C.w_router = I("w_router", [2, D, 72], F32)
    C.b_router = I("b_router", [2, 72], F32)
    if any(p.startswith("m2") for p in phases):
        C.w_gate = I("moe_w_gate", [2, 64, D, 512], F32)
        C.w_up = I("moe_w_up", [2, 64, D, 512], F32)
        C.w_down = I("moe_w_down", [2, 64, 512, D], F32)
    C.c_ident = I("c_ident", [128, 128], F32)
    C.c_tri = I("c_tri", [128, 128], F32)
    C.c_invf = I("c_invf", [128, 32], F32)
    C.c_band = I("c_band", [4, 3, 128, 128], F32)
    C.c_ebase = I("c_ebase", [128, 64], F32)
    C.out = _dram(nc, "out", [S, D], F32, "ExternalOutput")

    def scratch(name, shape, dt):
        return _dram(nc, name, shape, dt, "ExternalOutput" if name in debug_out else "Internal")
    C.Qd = scratch("Qd", [S, NH * 192], BF16)
    C.Kd = scratch("Kd", [S, NH * 192], BF16)
    C.Vd = scratch("Vd", [S, NH * 128], BF16)
    C.Od = scratch("Od", [S, D], BF16)
    C.x1d = scratch("x1d", [S, D], F32)
    C.x2d = scratch("x2d", [S, D], F32)
    C.x3d = scratch("x3d", [S, D], F32)
    C.xgd = scratch("xgd", [NSLOT + 128, D], BF16)
    C.ygd = scratch("ygd", [NSLOT + 128, D], BF16)
    C.zTd = scratch("zTd", [S, D], BF16)
    C.dbg = scratch("dbg", [S, 8], F32)
    C.has_moe = any(p.startswith("m2") for p in phases)
    C.bg_items = []
    C.bg_hist = []
    C.r_wb = {}
    if C.has_moe:
        C.wgb = _dram(nc, "wgb", [2, 64, 128, 16 * 512], BF16, "Internal")
        C.wub = _dram(nc, "wub", [2, 64, 128, 16 * 512], BF16, "Internal")
        C.wdb = _dram(nc, "wdb", [2, 64, 128, 4 * D], BF16, "Internal")
        for layer, ne in ((0, NCONV0), (1, NCONV1)):
            for ex in range(ne):
                for which in range(3):
                    C.bg_items.append((layer, ex, which))
                    C.r_wb[(layer, ex, which)] = Reg("wb")

    with contextlib.ExitStack() as es:
        P = Rec(nc, es)
        C.P = P
        C.ps = [es.enter_context(nc.psum_tensor("ps%d" % i, [128, 512], F32)) for i in range(8)]
        C.psr = [Reg("ps%d" % i) for i in range(8)]
        C.ps_rr = 0
        C.tr_rr = 0
        C.slot_i = es.enter_context(nc.sbuf_tensor("slot_i", [128, NT, 2], I32))
        C.gate_w = es.enter_context(nc.sbuf_tensor("gate_w", [128, NT, 2], F32))
        C.r_slot = [Reg() for _ in range(NT)]
        for nm in ("x", "Qd", "Kd", "Vd", "Od", "x1d", "x2d", "x3d", "zTd", "out"):
            setattr(C, "r_" + nm, [Reg(nm + str(i)) for i in range(NT)])
        C.r_xg = Reg("xg")
        C.r_ygt = Reg("ygt")
        C.r_xgt = [Reg("xgt%d" % i) for i in range(NT)]
        C.r_yg = [Reg("yg%d" % e) for e in range(64)]
        C.r_xge = [Reg("xge%d" % e) for e in range(64)]
        for ph in phases:
            with contextlib.ExitStack() as pes:
                if ph == "a1":
                    phase_a1(C, pes)
                elif ph == "a2":
                    phase_a2(C, pes)
                elif ph == "a3":
                    phase_proj_moe_front(C, pes, layer=0)
                elif ph == "m2_0":
                    phase_experts(C, pes, layer=0)
                elif ph == "m3_0":
                    phase_combine(C, pes, layer=0)
                elif ph == "l1a":
                    phase_pool_a(C, pes)
                elif ph == "l1b":
                    phase_proj_moe_front(C, pes, layer=1)
                elif ph == "m2_1":
                    phase_experts(C, pes, layer=1)
                elif ph == "m3_1":
                    phase_combine(C, pes, layer=1)
                P.drain()
                P.emit(ph)
    return nc


def next_bank(C):
    pool = getattr(C, "bank_pool", None) or list(range(8))
    b = pool[C.ps_rr % len(pool)]
    C.ps_rr += 1
    return C.ps[b], C.psr[b]


_UNIQ = [0]


def sb(pes, nc, name, shape, dt):
    _UNIQ[0] += 1
    return pes.enter_context(nc.sbuf_tensor("%s_u%d" % (name, _UNIQ[0]), list(shape), dt))


def load_bcast(C, q, dst, src_row, n, reg):
    C.P.dma(q, lambda e: e.dma_start(out=dst, in_=src_row.broadcast_to([128, n])), writes=[reg])


def load_w_cast(C, dst, src, reg, kc):
    n = src.shape[1]
    if n <= 2048:
        for c0 in range(0, kc, 8):
            c1 = min(kc, c0 + 8)
            C.P.dma("pool", lambda e, c0=c0, c1=c1: e.dma_start(
                out=dst[:, c0:c1, :], in_=src[c0 * 128:c1 * 128, :].rearrange("(c p) n -> p c n", p=128)), writes=[reg])
    else:
        for c in range(kc):
            C.P.dma("pool", lambda e, c=c: e.dma_start(
                out=dst[:, c, :], in_=src[c * 128:(c + 1) * 128, :], max_dma_last_dim=8192), writes=[reg])


def transposes_bf16(C, src_fn, n, dst, dst_reg, src_regs, width=128, evac=("act", "dve"), group=8, regions=None):
    P = C.P
    for gi, g0 in enumerate(range(0, n, group)):
        g1 = min(n, g0 + group)
        if regions is None:
            bank, breg = next_bank(C)
            bv = bank[:].bitcast(BF16).rearrange("p (j t) -> p j t", t=128)
        else:
            bv, breg = regions[C.tr_rr % len(regions)]
            C.tr_rr += 1
        for j in range(g0, g1):
            P.op("pe", lambda e: e.transpose(out=bv[:width, j - g0, :], in_=src_fn(j), identity=C.identb[:]),
                 reads=list(src_regs) + [C.r_ident], writes=[breg], sig=(j == g1 - 1))
        en = evac[gi % len(evac)]
        if en == "act":
            P.op("act", lambda e: e.activation(out=dst[:width, g0:g1, :], in_=bv[:width, 0:g1 - g0, :], func=AF.Copy),
                 reads=[breg], writes=[dst_reg])
        else:
            P.op("dve", lambda e: e.tensor_copy(out=dst[:width, g0:g1, :], in_=bv[:width, 0:g1 - g0, :]),
                 reads=[breg], writes=[dst_reg])


def transposes_bf16_gen(C, src_fn, n, dst, dst_reg, src_regs, width=128, evac=("act", "dve"), group=8, regions=None):
    P = C.P
    for gi, g0 in enumerate(range(0, n, group)):
        g1 = min(n, g0 + group)
        if regions is None:
            bank, breg = next_bank(C)
            bv = bank[:].bitcast(BF16).rearrange("p (j t) -> p j t", t=128)
        else:
            bv, breg = regions[C.tr_rr % len(regions)]
            C.tr_rr += 1
        for j in range(g0, g1):
            P.op("pe", lambda e: e.transpose(out=bv[:width, j - g0, :], in_=src_fn(j), identity=C.identb[:]),
                 reads=list(src_regs) + [C.r_ident], writes=[breg], sig=(j == g1 - 1))
        en = evac[gi % len(evac)]
        if en == "act":
            P.op("act", lambda e: e.activation(out=dst[:width, g0:g1, :], in_=bv[:width, 0:g1 - g0, :], func=AF.Copy),
                 reads=[breg], writes=[dst_reg])
        else:
            P.op("dve", lambda e: e.tensor_copy(out=dst[:width, g0:g1, :], in_=bv[:width, 0:g1 - g0, :]),
                 reads=[breg], writes=[dst_reg])
        yield


def rstd_from_ss(C, ss, rs, n, reg_ss, reg_rs, width):
    P = C.P
    P.op("act", lambda e: e.activation(out=rs, in_=ss, func=AF.Sqrt, scale=1.0 / n, bias=C.eps_t[:, 0:1]),
         reads=[reg_ss, C.r_const], writes=[reg_rs])
    P.op("dve", lambda e: e.reciprocal(out=rs, in_=rs), reads=[reg_rs], writes=[reg_rs])


def interleave(genB, genA, ratio):
    k = 0
    for _ in genB:
        k += 1
        if genA is not None and k % ratio == 0:
            if next(genA, "end") == "end":
                genA = None
    if genA is not None:
        for _ in genA:
            pass


def load_consts(C, pes):
    nc, P = C.nc, C.P
    C.identf = sb(pes, nc, "identf", [128, 128], F32)
    C.identb = sb(pes, nc, "identb", [128, 128], BF16)
    C.eps_t = sb(pes, nc, "eps_t", [128, 1], F32)
    C.r_ident = Reg("ident")
    C.r_const = Reg("const")
    P.dma("sp", lambda e: e.dma_start(out=C.identf[:], in_=C.c_ident), writes=[C.r_ident])
    P.op("dve", lambda e: e.tensor_copy(out=C.identb[:], in_=C.identf[:]), reads=[C.r_ident], writes=[C.r_ident])
    P.op("dve", lambda e: e.memset(C.eps_t[:], EPS), writes=[C.r_const])


def phase_a1(C, pes):
    nc, P = C.nc, C.P
    load_consts(C, pes)
    w_in = sb(pes, nc, "a1_w_in", [128, 16, 1088], BF16)
    wq = sb(pes, nc, "a1_wq", [128, 4, 3072], BF16)
    wkv = sb(pes, nc, "a1_wkv", [128, 4, 4096], BF16)
    g_mix = sb(pes, nc, "a1_gmix", [128, D], F32)
    g_lat = sb(pes, nc, "a1_glat", [128, 1024], F32)
    g_qn = sb(pes, nc, "a1_gqn", [128, 192], F32)
    g_kn = sb(pes, nc, "a1_gkn", [128, 192], F32)
    invf = sb(pes, nc, "a1_invf", [128, 32], F32)
    pos_i = sb(pes, nc, "a1_posi", [128, NT], I32)
    pos_f = sb(pes, nc, "a1_posf", [128, NT], F32)
    NB = 2
    xt = [sb(pes, nc, "a1_xt%d" % i, [128, D], F32) for i in range(NB)]
    r_xt = [Reg() for _ in range(NB)]
    ang = xt[1][:, 0:1024].rearrange("p (a b) -> p a b", b=32)
    kk_f = xt[1][:, 1024:2048].rearrange("p (a b) -> p a b", b=32)
    kk_i = xt[0][:, 0:1024].bitcast(I32).rearrange("p (a b) -> p a b", b=32)
    msk = xt[0][:, 1024:2048].rearrange("p (a b) -> p a b", b=32)
    sin_t = sb(pes, nc, "a1_sin", [128, NT, 32], F32)
    cos_t = sb(pes, nc, "a1_cos", [128, NT, 32], F32)
    r_w = Reg("a1w")
    r_g = Reg("a1g")
    r_rope = Reg("rope")
    load_w_cast(C, w_in, C.mla_w_in, r_w, 16)
    load_bcast(C, "sp", g_mix[:], C.mix_norm[0:1, :], D, r_g)
    load_bcast(C, "sp", g_lat[:, 0:512], C.q_lat[0:1, :], 512, r_g)
    load_bcast(C, "sp", g_lat[:, 512:1024], C.kv_lat[0:1, :], 512, r_g)
    load_bcast(C, "sp", g_qn[:], C.q_norm[0:1, :], 192, r_g)
    load_bcast(C, "sp", g_kn[:], C.k_norm[0:1, :], 192, r_g)
    P.dma("sp", lambda e: e.dma_start(out=invf[:], in_=C.c_invf), writes=[r_rope])
    P.dma("sp", lambda e: e.dma_start(out=pos_i[:], in_=C.pos), writes=[r_rope])
    load_w_cast(C, wq, C.w_q_up, r_w, 4)
    load_w_cast(C, wkv, C.w_kv_up, r_w, 4)
    P.op("dve", lambda e: e.tensor_copy(out=pos_f[:], in_=pos_i[:]), reads=[r_rope], writes=[r_rope, r_xt[0], r_xt[1]])
    P.op("dve", lambda e: e.tensor_tensor(out=ang[:], in0=pos_f[:, :].unsqueeze(2).broadcast_to([128, NT, 32]),
                                          in1=invf[:, :].unsqueeze(1).broadcast_to([128, NT, 32]), op=ALU.mult),
         reads=[r_rope], writes=[r_rope, r_xt[0], r_xt[1]])
    P.op("dve", lambda e: e.tensor_scalar(out=kk_f[:], in0=ang[:], scalar1=1.0 / TWO_PI, scalar2=0.5, op0=ALU.mult, op1=ALU.add),
         reads=[r_rope], writes=[r_rope, r_xt[0], r_xt[1]])
    P.op("dve", lambda e: e.tensor_copy(out=kk_i[:], in_=kk_f[:]), reads=[r_rope], writes=[r_rope, r_xt[0], r_xt[1]])
    P.op("dve", lambda e: e.tensor_copy(out=kk_f[:], in_=kk_i[:]), reads=[r_rope], writes=[r_rope, r_xt[0], r_xt[1]])
    C1 = 6.28125
    C2 = TWO_PI - C1
    P.op("dve", lambda e: e.scalar_tensor_tensor(out=ang[:], in0=kk_f[:], scalar=-C1, in1=ang[:], op0=ALU.mult, op1=ALU.add),
         reads=[r_rope], writes=[r_rope, r_xt[0], r_xt[1]])
    P.op("dve", lambda e: e.scalar_tensor_tensor(out=ang[:], in0=kk_f[:], scalar=-C2, in1=ang[:], op0=ALU.mult, op1=ALU.add),
         reads=[r_rope], writes=[r_rope, r_xt[0], r_xt[1]])
    def wrap(buf, lo, hi):
        if lo:
            P.op("dve", lambda e: e.tensor_single_scalar(out=msk[:], in_=buf[:], scalar=-PI, op=ALU.is_lt), reads=[r_rope], writes=[r_rope, r_xt[0], r_xt[1]])
            P.op("dve", lambda e: e.scalar_tensor_tensor(out=buf[:], in0=msk[:], scalar=TWO_PI, in1=buf[:], op0=ALU.mult, op1=ALU.add), reads=[r_rope], writes=[r_rope, r_xt[0], r_xt[1]])
        if hi:
            P.op("dve", lambda e: e.tensor_single_scalar(out=msk[:], in_=buf[:], scalar=PI, op=ALU.is_gt), reads=[r_rope], writes=[r_rope, r_xt[0], r_xt[1]])
            P.op("dve", lambda e: e.scalar_tensor_tensor(out=buf[:], in0=msk[:], scalar=-TWO_PI, in1=buf[:], op0=ALU.mult, op1=ALU.add), reads=[r_rope], writes=[r_rope, r_xt[0], r_xt[1]])
    wrap(ang, True, True)
    P.op("act", lambda e: e.activation(out=sin_t[:], in_=ang[:], func=AF.Sin), reads=[r_rope], writes=[r_rope, r_xt[0], r_xt[1]])
    P.op("dve", lambda e: e.tensor_scalar(out=ang[:], in0=ang[:], scalar1=PI / 2, scalar2=None, op0=ALU.add), reads=[r_rope], writes=[r_rope, r_xt[0], r_xt[1]])
    wrap(ang, False, True)
    P.op("act", lambda e: e.activation(out=cos_t[:], in_=ang[:], func=AF.Sin), reads=[r_rope], writes=[r_rope, r_xt[0], r_xt[1]])

    junk = sb(pes, nc, "a1_junk", [128, D], BF16)
    r_junk = Reg()
    hb2 = [sb(pes, nc, "a1_hb%d" % i, [128, D], BF16) for i in range(2)]
    r_hb2 = [Reg() for _ in range(2)]
    hT2 = [sb(pes, nc, "a1_hT%d" % i, [128, 16, 128], BF16) for i in range(2)]
    r_hT2 = [Reg() for _ in range(2)]
    cn2 = [sb(pes, nc, "a1_cn%d" % i, [128, 1024], BF16) for i in range(2)]
    r_cn2 = [Reg() for _ in range(2)]
    cT2 = [sb(pes, nc, "a1_cT%d" % i, [128, 8, 128], BF16) for i in range(2)]
    r_cT2 = [Reg() for _ in range(2)]
    st2 = [sb(pes, nc, "a1_st%d" % i, [128, 64], F32) for i in range(2)]
    r_st2 = [[Reg() for _ in range(4)] for _ in range(2)]
    ssq2 = [sb(pes, nc, "a1_ssq%d" % i, [128, 16], F32) for i in range(2)]
    rq2 = [sb(pes, nc, "a1_rq%d" % i, [128, 16], F32) for i in range(2)]
    ssk2 = [sb(pes, nc, "a1_ssk%d" % i, [128, 16], F32) for i in range(2)]
    rk2 = [sb(pes, nc, "a1_rk%d" % i, [128, 16], F32) for i in range(2)]
    r_sq2 = [[Reg() for _ in range(8)] for _ in range(2)]
    r_sk2 = [[Reg() for _ in range(8)] for _ in range(2)]
    krg2 = [sb(pes, nc, "a1_krg%d" % i, [128, 64], F32) for i in range(2)]
    krr2 = [sb(pes, nc, "a1_krr%d" % i, [128, 64], F32) for i in range(2)]
    tA1 = sb(pes, nc, "a1_tA", [128, 16, 64], F32)
    tB1 = sb(pes, nc, "a1_tB", [128, 16, 64], F32)
    tA2 = [tA1, tA1]
    tB2 = [tB1, tB1]
    tK2 = [sb(pes, nc, "a1_tK%d" % i, [128, 2, 64], F32) for i in range(2)]
    r_kr2 = [Reg() for _ in range(2)]
    r_t1 = Reg()
    r_t2 = [r_t1, r_t1]
    r_tk2 = [Reg() for _ in range(2)]
    qn = [sb(pes, nc, "a1_qn%d" % i, [128, 16, 192], BF16) for i in range(NB)]
    kn = [sb(pes, nc, "a1_kn%d" % i, [128, 16, 192], BF16) for i in range(NB)]
    vv = [sb(pes, nc, "a1_vv%d" % i, [128, 16, 128], BF16) for i in range(NB)]
    r_qn = [Reg() for _ in range(NB)]
    r_kn = [Reg() for _ in range(NB)]
    r_vv = [Reg() for _ in range(NB)]

    def load_x(i):
        b = i % NB
        P.dma("sp", lambda e: e.dma_start(out=xt[b][:], in_=C.x[i * 128:(i + 1) * 128, :]), reads=[C.r_x[i]], writes=[r_xt[b]])

    def stageA(i):
        b = i % NB
        if i + 1 < NT:
            load_x(i + 1)
        X = xt[b]
        hb, r_hb, hT, r_hT, cn, r_cn, cT, r_cT, st = hb2[b], r_hb2[b], hT2[b], r_hT2[b], cn2[b], r_cn2[b], cT2[b], r_cT2[b], st2[b]
        r_st, r_stl, r_str = r_st2[b][0], r_st2[b][1], r_st2[b][2]
        ssq, rq, ssk, rk, r_sq, r_sk = ssq2[b], rq2[b], ssk2[b], rk2[b], r_sq2[b], r_sk2[b]
        krg, krr, tA, tB, tK, r_kr, r_t, r_tk = krg2[b], krr2[b], tA2[b], tB2[b], tK2[b], r_kr2[b], r_t2[b], r_tk2[b]
        P.op("act", lambda e, X=X: e.activation(out=junk[:], in_=X[:], func=AF.Square, accum_out=st[:, 0:1]), reads=[r_xt[b]], writes=[r_st])
        rstd_from_ss(C, st[:, 0:1], st[:, 1:2], D, r_st, r_st, 1)
        P.op("dve", lambda e, X=X: e.scalar_tensor_tensor(out=hb[:], in0=X[:], scalar=st[:, 1:2], in1=g_mix[:], op0=ALU.mult, op1=ALU.mult),
             reads=[r_xt[b], r_st, r_g], writes=[r_hb])
        yield
        transposes_bf16(C, lambda j: hb[:, j * 128:(j + 1) * 128], 16, hT, r_hT, [r_hb])
        yield
        slabs = [(0, 512), (512, 512), (1024, 64)]
        cb = []
        for (o, n) in slabs:
            bank, breg = next_bank(C)
            cb.append((bank, breg))
            for c in range(16):
                P.op("pe", lambda e, bank=bank, c=c, o=o, n=n: e.matmul(out=bank[:, 0:n], lhsT=hT[:, c, :], rhs=w_in[:, c, o:o + n], start=(c == 0), stop=(c == 15)),
                     reads=[r_hT, r_w], writes=[breg], sig=(c == 15))
            yield
        for s_ in range(2):
            bank, breg = cb[s_]
            P.op("act", lambda e, bank=bank, s_=s_: e.activation(out=junk[:, 0:512], in_=bank[:, :], func=AF.Square, accum_out=st[:, 2 + s_:3 + s_]),
                 reads=[breg], writes=[r_stl])
        rstd_from_ss(C, st[:, 2:4], st[:, 4:6], 512, r_stl, r_stl, 2)
        for s_ in range(2):
            bank, breg = cb[s_]
            P.op("dve", lambda e, bank=bank, s_=s_: e.scalar_tensor_tensor(out=cn[:, s_ * 512:(s_ + 1) * 512], in0=bank[:, :], scalar=st[:, 4 + s_:5 + s_],
                                                                           in1=g_lat[:, s_ * 512:(s_ + 1) * 512], op0=ALU.mult, op1=ALU.mult),
                 reads=[breg, r_stl, r_g], writes=[r_cn])
        yield
        bank, breg = cb[2]
        P.op("act", lambda e, bank=bank: e.activation(out=junk[:, 0:64], in_=bank[:, 0:64], func=AF.Square, accum_out=st[:, 6:7]),
             reads=[breg], writes=[r_str])
        P.op("dve", lambda e, bank=bank: e.tensor_tensor(out=krg[:], in0=bank[:, 0:64], in1=g_kn[:, 128:192], op=ALU.mult),
             reads=[breg, r_g], writes=[r_kr])
        cosb = cos_t[:, i, :].unsqueeze(1).broadcast_to([128, 2, 32])
        sinb = sin_t[:, i, :].unsqueeze(1).broadcast_to([128, 2, 32])
        krg3 = krg[:, :].rearrange("p (t d) -> p t d", t=2)
        tA0 = tK[:, 0, :].rearrange("p (t d) -> p t d", t=2)
        tB0 = tK[:, 1, :].rearrange("p (t d) -> p t d", t=2)
        P.op("dve", lambda e: e.tensor_tensor(out=tA0, in0=krg3, in1=cosb, op=ALU.mult), reads=[r_kr, r_rope], writes=[r_tk])
        P.op("dve", lambda e: e.tensor_tensor(out=tB0, in0=krg3, in1=sinb, op=ALU.mult), reads=[r_kr, r_rope], writes=[r_tk])
        P.op("dve", lambda e: e.tensor_tensor(out=krr[:, 0:32], in0=tK[:, 0, 0:32], in1=tK[:, 1, 32:64], op=ALU.subtract), reads=[r_tk], writes=[r_kr])
        P.op("dve", lambda e: e.tensor_tensor(out=krr[:, 32:64], in0=tK[:, 0, 32:64], in1=tK[:, 1, 0:32], op=ALU.add), reads=[r_tk], writes=[r_kr])
        yield
        transposes_bf16(C, lambda j: cn[:, j * 128:(j + 1) * 128], 8, cT, r_cT, [r_cn])

    def stageB(i):
        b = i % NB
        X = xt[b]
        hb, r_hb, hT, r_hT, cn, r_cn, cT, r_cT, st = hb2[b], r_hb2[b], hT2[b], r_hT2[b], cn2[b], r_cn2[b], cT2[b], r_cT2[b], st2[b]
        r_st, r_stl, r_str = r_st2[b][0], r_st2[b][1], r_st2[b][2]
        ssq, rq, ssk, rk, r_sq, r_sk = ssq2[b], rq2[b], ssk2[b], rk2[b], r_sq2[b], r_sk2[b]
        krg, krr, tA, tB, tK, r_kr, r_t, r_tk = krg2[b], krr2[b], tA2[b], tB2[b], tK2[b], r_kr2[b], r_t2[b], r_tk2[b]
        Q, K, V = qn[b], kn[b], vv[b]
        bg_issue(C, 2, after=[r_cT])
        for s_ in range(8):
            bank, breg = next_bank(C)
            for c in range(4):
                P.op("pe", lambda e, bank=bank, c=c, s_=s_: e.matmul(out=bank[:, 0:384], lhsT=cT[:, c, :], rhs=wq[:, c, s_ * 384:(s_ + 1) * 384], start=(c == 0), stop=(c == 3)),
                     reads=[r_cT, r_w], writes=[breg], sig=(c == 3))
            for hh in range(2):
                hd = 2 * s_ + hh
                P.op("act", lambda e, bank=bank, hh=hh, hd=hd: e.activation(out=junk[:, 0:192], in_=bank[:, hh * 192:(hh + 1) * 192], func=AF.Square, accum_out=ssq[:, hd:hd + 1]),
                     reads=[breg], writes=[r_sq[s_]])
            rstd_from_ss(C, ssq[:, 2 * s_:2 * s_ + 2], rq[:, 2 * s_:2 * s_ + 2], 192, r_sq[s_], r_sq[s_], 2)
            for hh in range(2):
                hd = 2 * s_ + hh
                P.op("dve", lambda e, bank=bank, hh=hh, hd=hd, Q=Q: e.scalar_tensor_tensor(out=Q[:, hd, :], in0=bank[:, hh * 192:(hh + 1) * 192], scalar=rq[:, hd:hd + 1],
                                                                                          in1=g_qn[:], op0=ALU.mult, op1=ALU.mult),
                     reads=[breg, r_sq[s_], r_g], writes=[r_qn[b]])
            yield
        cos16 = cos_t[:, i, :].unsqueeze(1).unsqueeze(1).broadcast_to([128, 16, 2, 32])
        sin16 = sin_t[:, i, :].unsqueeze(1).unsqueeze(1).broadcast_to([128, 16, 2, 32])
        qr4 = Q[:, :, 128:192].rearrange("p h (t d) -> p h t d", t=2)
        tA4 = tA[:, :, :].rearrange("p h (t d) -> p h t d", t=2)
        tB4 = tB[:, :, :].rearrange("p h (t d) -> p h t d", t=2)
        P.op("dve", lambda e: e.tensor_tensor(out=tA4, in0=qr4, in1=cos16, op=ALU.mult), reads=[r_qn[b], r_rope], writes=[r_t])
        P.op("dve", lambda e: e.tensor_tensor(out=tB4, in0=qr4, in1=sin16, op=ALU.mult), reads=[r_qn[b], r_rope], writes=[r_t])
        P.op("dve", lambda e, Q=Q: e.tensor_tensor(out=Q[:, :, 128:160], in0=tA[:, :, 0:32], in1=tB[:, :, 32:64], op=ALU.subtract), reads=[r_t], writes=[r_qn[b]])
        P.op("dve", lambda e, Q=Q: e.tensor_tensor(out=Q[:, :, 160:192], in0=tA[:, :, 32:64], in1=tB[:, :, 0:32], op=ALU.add), reads=[r_t], writes=[r_qn[b]])
        P.dma("sp", lambda e, Q=Q, i=i: e.dma_start(out=C.Qd[i * 128:(i + 1) * 128, :], in_=Q[:, :, :].rearrange("p h d -> p (h d)")),
              reads=[r_qn[b]], writes=[C.r_Qd[i]])
        yield
        for s_ in range(8):
            bank, breg = next_bank(C)
            for c in range(4):
                P.op("pe", lambda e, bank=bank, c=c, s_=s_: e.matmul(out=bank[:, :], lhsT=cT[:, 4 + c, :], rhs=wkv[:, c, s_ * 512:(s_ + 1) * 512], start=(c == 0), stop=(c == 3)),
                     reads=[r_cT, r_w], writes=[breg], sig=(c == 3))
            b4 = bank[:, :].rearrange("p (h t d) -> p h t d", h=2, t=2)
            for hh in range(2):
                hd = 2 * s_ + hh
                P.op("act", lambda e, b4=b4, hh=hh, hd=hd: e.activation(out=junk[:, 0:128], in_=b4[:, hh, 0, :], func=AF.Square, accum_out=ssk[:, hd:hd + 1]),
                     reads=[breg], writes=[r_sk[s_]])
            P.op("act", lambda e, b4=b4, s_=s_, V=V: e.activation(out=V[:, 2 * s_:2 * s_ + 2, :], in_=b4[:, :, 1, :], func=AF.Copy), reads=[breg], writes=[r_vv[b]])
            P.op("dve", lambda e, s_=s_: e.tensor_scalar(out=ssk[:, 2 * s_:2 * s_ + 2], in0=ssk[:, 2 * s_:2 * s_ + 2], scalar1=st[:, 6:7], scalar2=None, op0=ALU.add),
                 reads=[r_sk[s_], r_str], writes=[r_sk[s_]])
            rstd_from_ss(C, ssk[:, 2 * s_:2 * s_ + 2], rk[:, 2 * s_:2 * s_ + 2], 192, r_sk[s_], r_sk[s_], 2)
            for hh in range(2):
                hd = 2 * s_ + hh
                P.op("dve", lambda e, b4=b4, hh=hh, hd=hd, K=K: e.scalar_tensor_tensor(out=K[:, hd, 0:128], in0=b4[:, hh, 0, :], scalar=rk[:, hd:hd + 1],
                                                                                      in1=g_kn[:, 0:128], op0=ALU.mult, op1=ALU.mult),
                     reads=[breg, r_sk[s_], r_g], writes=[r_kn[b]])
            yield
        P.op("dve", lambda e: e.tensor_tensor(out=K[:, :, 128:192], in0=krr[:, :].unsqueeze(1).broadcast_to([128, 16, 64]),
                                              in1=rk[:, :].unsqueeze(2).broadcast_to([128, 16, 64]), op=ALU.mult),
             reads=[r_kr] + list(r_sk), writes=[r_kn[b]])
        P.dma("sp", lambda e, K=K, i=i: e.dma_start(out=C.Kd[i * 128:(i + 1) * 128, :], in_=K[:, :, :].rearrange("p h d -> p (h d)")),
              reads=[r_kn[b]], writes=[C.r_Kd[i]])
        P.dma("sp", lambda e, V=V, i=i: e.dma_start(out=C.Vd[i * 128:(i + 1) * 128, :], in_=V[:, :, :].rearrange("p h d -> p (h d)")),
              reads=[r_vv[b]], writes=[C.r_Vd[i]])

    load_x(0)
    for _ in stageA(0):
        pass
    for i in range(NT):
        interleave(stageB(i), stageA(i + 1) if i + 1 < NT else None, 2)


A2_SPLIT = 96
A2_REGIONS = False


def phase_a2(C, pes):
    nc, P = C.nc, C.P
    load_consts(C, pes)
    tb = C.ps[7][:].bitcast(BF16).rearrange("p (r j t) -> p r j t", r=2, t=128)
    tr_regions = [(tb[:, 0], Reg("trh0")), (tb[:, 1], Reg("trh1"))]
    if not A2_REGIONS:
        C.bank_pool = [7]
    Qh = [sb(pes, nc, "a2_Q%d" % i, [128, NT, 192], BF16) for i in range(2)]
    Kh = [sb(pes, nc, "a2_K%d" % i, [128, NT, 192], BF16) for i in range(2)]
    Vh = [sb(pes, nc, "a2_V%d" % i, [128, NT, 132], BF16) for i in range(2)]
    W1 = A2_SPLIT
    W2 = 192 - W1
    QTn = [sb(pes, nc, "a2_QTn%d" % i, [W1, NT, 128], BF16) for i in range(2)]
    QTr = [sb(pes, nc, "a2_QTr%d" % i, [W2, NT, 128], BF16) for i in range(2)]
    KTn = [sb(pes, nc, "a2_KTn%d" % i, [W1, NT, 128], BF16) for i in range(2)]
    KTr = [sb(pes, nc, "a2_KTr%d" % i, [W2, NT, 128], BF16) for i in range(2)]
    Oh = [sb(pes, nc, "a2_O%d" % i, [128, NT, 128], BF16) for i in range(2)]
    NR = 6
    PT = [sb(pes, nc, "a2_PT%d" % i, [128, 512], BF16) for i in range(NR)]
    ND = 6
    PTd = [sb(pes, nc, "a2_PTd%d" % i, [128, 512], BF16) for i in range(ND)]
    r_PTd = [Reg() for _ in range(ND)]
    for i in range(ND):
        P.op("pool", lambda e: e.memset(PTd[i][:], 0.0), writes=[r_PTd[i]])
    dctr = [0]
    rsum = sb(pes, nc, "a2_rs", [128, 8], F32)
    r_Q = [Reg() for _ in range(2)]
    r_K = [Reg() for _ in range(2)]
    r_V = [Reg() for _ in range(2)]
    r_QT = [Reg() for _ in range(2)]
    r_KT = [Reg() for _ in range(2)]
    r_O = [Reg() for _ in range(2)]
    r_PT = [Reg() for _ in range(NR)]
    r_rs = [Reg() for _ in range(8)]
    NS = 3
    Sb = [C.ps[0], C.ps[1], C.ps[2]]
    r_S = [C.psr[0], C.psr[1], C.psr[2]]
    acc = [C.ps[3 + j] for j in range(4)]
    r_acc = [C.psr[3 + j] for j in range(4)]
    for b in range(2):
        P.op("pool", lambda e: e.memset(Vh[b][:, :, 128:129], 1.0), writes=[r_V[b]])
    scale = 192.0 ** -0.5

    def prologue(h):
        b = h % 2
        P.dma("sp", lambda e: e.dma_start(out=Qh[b][:], in_=C.Qd[:, h * 192:(h + 1) * 192].rearrange("(i p) d -> p i d", p=128)),
              reads=C.r_Qd, writes=[r_Q[b]])
        P.dma("sp", lambda e: e.dma_start(out=Kh[b][:], in_=C.Kd[:, h * 192:(h + 1) * 192].rearrange("(i p) d -> p i d", p=128)),
              reads=C.r_Kd, writes=[r_K[b]])
        P.dma("sp", lambda e: e.dma_start(out=Vh[b][:, :, 0:128], in_=C.Vd[:, h * 128:(h + 1) * 128].rearrange("(i p) d -> p i d", p=128)),
              reads=C.r_Vd, writes=[r_V[b]])
        kw = dict(group=4, regions=tr_regions) if A2_REGIONS else dict()
        yield from transposes_bf16_gen(C, lambda j: Qh[b][:, j, 0:W1], NT, QTn[b], r_QT[b], [r_Q[b]], width=W1, **kw)
        yield from transposes_bf16_gen(C, lambda j: Qh[b][:, j, W1:192], NT, QTr[b], r_QT[b], [r_Q[b]], width=W2, **kw)
        yield from transposes_bf16_gen(C, lambda j: Kh[b][:, j, 0:W1], NT, KTn[b], r_KT[b], [r_K[b]], width=W1, **kw)
        yield from transposes_bf16_gen(C, lambda j: Kh[b][:, j, W1:192], NT, KTr[b], r_KT[b], [r_K[b]], width=W2, **kw)

    def main(h):
        b = h % 2
        qtn = QTn[b][:, :, :].rearrange("p j t -> p (j t)")
        qtr = QTr[b][:, :, :].rearrange("p j t -> p (j t)")
        units = []
        for qg in range(8):
            for kt in range(4 * qg + 4):
                units.append((qg, kt))

        def qk(u):
            qg, kt = units[u]
            j0 = max(0, kt - 4 * qg)
            ncols = (4 - j0) * 128
            q0 = qg * 512 + j0 * 128
            sbk = Sb[u % NS]
            P.op("pe", lambda e: e.matmul(out=sbk[:, 0:ncols], lhsT=KTn[b][:, kt, :], rhs=qtn[:, q0:q0 + ncols], start=True, stop=False),
                 reads=[r_KT[b], r_QT[b]], writes=[r_S[u % NS]], sig=False)
            P.op("pe", lambda e: e.matmul(out=sbk[:, 0:ncols], lhsT=KTr[b][:, kt, :], rhs=qtr[:, q0:q0 + ncols], start=False, stop=True),
                 reads=[r_KT[b], r_QT[b]], writes=[r_S[u % NS]], sig=True)

        def rest(u):
            qg, kt = units[u]
            j0 = max(0, kt - 4 * qg)
            ncols = (4 - j0) * 128
            sbk = Sb[u % NS]
            if kt >= 4 * qg:
                r = dctr[0] % ND
                dctr[0] += 1
                PTu, rPTu = PTd[r], r_PTd[r]
                if ncols > 64:
                    P.op("act", lambda e: e.activation(out=PTu[:, 64:ncols], in_=sbk[:, 64:ncols], func=AF.Exp, scale=scale),
                         reads=[r_S[u % NS]], writes=[rPTu])
                P.op("act", lambda e: e.activation(out=PTu[0:64, 0:64], in_=sbk[0:64, 0:64], func=AF.Exp, scale=scale),
                     reads=[r_S[u % NS]], writes=[rPTu])
            else:
                r = u % NR
                PTu, rPTu = PT[r], r_PT[r]
                P.op("act", lambda e: e.activation(out=PTu[:, 0:ncols], in_=sbk[:, 0:ncols], func=AF.Exp, scale=scale),
                     reads=[r_S[u % NS]], writes=[rPTu])
            for jj in range(j0, 4):
                off = (jj - j0) * 128
                last = (kt == 4 * qg + jj)
                P.op("pe", lambda e: e.matmul(out=acc[jj][:, 0:129], lhsT=PTu[:, off:off + 128], rhs=Vh[b][:, kt, 0:129],
                                              start=(kt == 0), stop=last),
                     reads=[rPTu, r_V[b]], writes=[r_acc[jj]], sig=(last or jj == 3))
                if last:
                    qt = 4 * qg + jj
                    ri = qt % 8
                    P.op("dve", lambda e: e.reciprocal(out=rsum[:, ri:ri + 1], in_=acc[jj][:, 128:129]), reads=[r_acc[jj]], writes=[r_rs[ri]])
                    P.op("dve", lambda e: e.tensor_scalar(out=Oh[b][:, qt, :], in0=acc[jj][:, 0:128], scalar1=rsum[:, ri:ri + 1], scalar2=None, op0=ALU.mult),
                         reads=[r_acc[jj], r_rs[ri]], writes=[r_O[b]])
        qk(0)
        qk(1)
        for u in range(len(units)):
            if u + 2 < len(units):
                qk(u + 2)
            rest(u)
            if u % 36 == 18:
                bg_issue(C, 1, after=[r_S[u % NS]])
            yield
        P.dma("sp", lambda e: e.dma_start(out=C.Od[:, h * 128:(h + 1) * 128].rearrange("(i p) d -> p i d", p=128), in_=Oh[b][:]),
              reads=[r_O[b]], writes=C.r_Od)

    for _ in prologue(0):
        pass
    for h in range(NH):
        interleave(main(h), prologue(h + 1) if h + 1 < NH else None, 8)
    C.bank_pool = None


def phase_proj_moe_front(C, pes, layer):
    nc, P = C.nc, C.P
    load_consts(C, pes)
    if layer == 0:
        A_d, rA, W_d, Xin, rXin, Xout, rXout = C.Od, C.r_Od, C.mla_w_out, C.x, C.r_x, C.x1d, C.r_x1d
    else:
        A_d, rA, W_d, Xin, rXin, Xout, rXout = C.zTd, C.r_zTd, C.pool_w_out, C.x2d, C.r_x2d, C.x3d, C.r_x3d
    wo = sb(pes, nc, "pf_wo", [128, 16, D], BF16)
    r_w = Reg()
    load_w_cast(C, wo, W_d, r_w, 16)
    g_ffn = sb(pes, nc, "pf_g", [128, D], F32)
    wr = sb(pes, nc, "pf_wr", [128, 16, 72], F32)
    bias_b = sb(pes, nc, "pf_bias", [128, 72], F32)
    trif = sb(pes, nc, "pf_trif", [128, 128], F32)
    trib = sb(pes, nc, "pf_trib", [128, 128], BF16)
    onesb = sb(pes, nc, "pf_ones", [128, 128], BF16)
    ebase = sb(pes, nc, "pf_ebase", [128, 64], F32)
    trash_p = sb(pes, nc, "pf_trash", [128, 1], F32)
    trash_i = sb(pes, nc, "pf_trashi", [128, 1], I32)
    mcum = sb(pes, nc, "pf_mcum", [128, 64], F32)
    mcum_b = sb(pes, nc, "pf_mcumb", [128, 64], BF16)
    r_c = Reg()
    r_mc = Reg()
    load_bcast(C, "sp", g_ffn[:], C.ffn_norm[layer:layer + 1, :], D, r_c)
    load_bcast(C, "sp", bias_b[:], C.b_router[layer:layer + 1, :], 72, r_c)
    P.dma("sp", lambda e: e.dma_start(out=wr[:], in_=C.w_router[layer].rearrange("(c p) n -> p c n", p=128)), writes=[r_c])
    P.dma("sp", lambda e: e.dma_start(out=trif[:], in_=C.c_tri), writes=[r_c])
    P.dma("sp", lambda e: e.dma_start(out=ebase[:], in_=C.c_ebase), writes=[r_c])
    P.op("dve", lambda e: e.tensor_copy(out=trib[:], in_=trif[:]), reads=[r_c], writes=[r_c])
    P.op("dve", lambda e: e.memset(onesb[:], 1.0), writes=[r_c])
    P.op("pool", lambda e: e.iota(trash_i[:], pattern=[[0, 1]], base=TRASH, channel_multiplier=1), writes=[r_c])
    P.op("dve", lambda e: e.tensor_copy(out=trash_p[:], in_=trash_i[:]), reads=[r_c], writes=[r_c])
    P.op("dve", lambda e: e.memset(mcum[:], 0.0), writes=[r_mc])
    P.op("dve", lambda e: e.memset(mcum_b[:], 0.0), writes=[r_mc])

    NB = 2
    At = [sb(pes, nc, "pf_A%d" % i, [128, D], BF16) for i in range(NB)]
    Xt = [sb(pes, nc, "pf_X%d" % i, [128, D], F32) for i in range(NB)]
    r_A = [Reg() for _ in range(NB)]
    r_X = [Reg() for _ in range(NB)]
    aT = sb(pes, nc, "pf_aT", [128, 16, 128], BF16)
    r_aT = Reg()
    x1t = [sb(pes, nc, "pf_x1%d" % i, [128, D], F32) for i in range(NB)]
    r_x1 = [Reg() for _ in range(NB)]
    junk = sb(pes, nc, "pf_junk", [128, D], BF16)
    r_junk = Reg()
    hf2 = [sb(pes, nc, "pf_hf%d" % i, [128, D], F32) for i in range(2)]
    r_hf2 = [Reg() for _ in range(2)]
    stA2 = [sb(pes, nc, "pf_stA%d" % i, [128, 2], F32) for i in range(2)]
    r_stA2 = [Reg() for _ in range(2)]
    hb = [sb(pes, nc, "pf_hb%d" % i, [128, D], BF16) for i in range(NB)]
    r_hb = [Reg() for _ in range(NB)]
    hT = sb(pes, nc, "pf_hT", [128, 16, 128], F32)
    r_hT = Reg()
    st = sb(pes, nc, "pf_st", [128, 32], F32)
    r_st = Reg()
    L = sb(pes, nc, "pf_L", [128, 72], F32)
    t8 = sb(pes, nc, "pf_t8", [128, 8, 8], F32)
    t64 = sb(pes, nc, "pf_t64", [128, 8, 8], F32)
    M1 = sb(pes, nc, "pf_M1", [128, 8, 8], F32)
    M2 = sb(pes, nc, "pf_M2", [128, 8, 8], F32)
    Mb = sb(pes, nc, "pf_Mb", [128, 64], BF16)
    sv = sb(pes, nc, "pf_sv", [128, 64], F32)
    r_r = Reg()

    def load(i):
        b = i % NB
        P.dma("sp", lambda e: e.dma_start(out=At[b][:], in_=A_d[i * 128:(i + 1) * 128, :]), reads=[rA[i]], writes=[r_A[b]])
        P.dma("sp", lambda e: e.dma_start(out=Xt[b][:], in_=Xin[i * 128:(i + 1) * 128, :]), reads=[rXin[i]], writes=[r_X[b]])

    def dv(fn, reads=(), writes=()):
        P.op("dve", fn, reads=[r_r] + list(reads), writes=[r_r] + list(writes))

    def stageA(i):
        b = i % NB
        hf, r_hf, stA, r_stA = hf2[b], r_hf2[b], stA2[b], r_stA2[b]
        if i + 1 < NT:
            load(i + 1)
        transposes_bf16(C, lambda j: At[b][:, j * 128:(j + 1) * 128], 16, aT, r_aT, [r_A[b]])
        yield
        for s_ in range(4):
            bank, breg = next_bank(C)
            for c in range(16):
                P.op("pe", lambda e: e.matmul(out=bank[:, :], lhsT=aT[:, c, :], rhs=wo[:, c, s_ * 512:(s_ + 1) * 512], start=(c == 0), stop=(c == 15)),
                     reads=[r_aT, r_w], writes=[breg], sig=(c == 15))
            P.op("dve", lambda e: e.tensor_tensor(out=x1t[b][:, s_ * 512:(s_ + 1) * 512], in0=bank[:, :], in1=Xt[b][:, s_ * 512:(s_ + 1) * 512], op=ALU.add),
                 reads=[breg, r_X[b]], writes=[r_x1[b]])
            yield
        P.dma("sp", lambda e: e.dma_start(out=Xout[i * 128:(i + 1) * 128, :], in_=x1t[b][:]), reads=[r_x1[b]], writes=[rXout[i]])
        P.op("act", lambda e: e.activation(out=junk[:], in_=x1t[b][:], func=AF.Square, accum_out=stA[:, 0:1]), reads=[r_x1[b]], writes=[r_stA])
        rstd_from_ss(C, stA[:, 0:1], stA[:, 1:2], D, r_stA, r_stA, 1)
        P.op("dve", lambda e: e.scalar_tensor_tensor(out=hf[:], in0=x1t[b][:], scalar=stA[:, 1:2], in1=g_ffn[:], op0=ALU.mult, op1=ALU.mult),
             reads=[r_x1[b], r_stA, r_c], writes=[r_hf])
        P.op("act", lambda e: e.activation(out=hb[b][:], in_=hf[:], func=AF.Copy), reads=[r_hf], writes=[r_hb[b]])
        yield

    def stageB(i):
        b = i % NB
        hf, r_hf = hf2[b], r_hf2[b]
        bg_issue(C, 2, after=[r_hf])
        for g0 in range(0, 16, 4):
            bank, breg = next_bank(C)
            for j in range(4):
                c = g0 + j
                P.op("pe", lambda e: e.transpose(out=bank[:, j * 128:(j + 1) * 128], in_=hf[:, c * 128:(c + 1) * 128], identity=C.identf[:]),
                     reads=[r_hf, C.r_ident], writes=[breg], sig=(j == 3))
            if (g0 // 4) % 2 == 0:
                P.op("act", lambda e: e.activation(out=hT[:, g0:g0 + 4, :], in_=bank[:, :].rearrange("p (j t) -> p j t", t=128), func=AF.Copy), reads=[breg], writes=[r_hT])
            else:
                P.op("dve", lambda e: e.tensor_copy(out=hT[:, g0:g0 + 4, :], in_=bank[:, :].rearrange("p (j t) -> p j t", t=128)), reads=[breg], writes=[r_hT])
        yield
        lbank, lreg = next_bank(C)
        for c in range(16):
            P.op("pe", lambda e: e.matmul(out=lbank[:, 0:72], lhsT=hT[:, c, :], rhs=wr[:, c, :], start=(c == 0), stop=(c == 15)),
                 reads=[r_hT, r_c], writes=[lreg], sig=(c == 15))
        yield
        G = L[:, 0:8]
        LE = L[:, 8:72].rearrange("p (g e) -> p g e", g=8)
        gm, ngm, sg, psel = st[:, 2:3], st[:, 3:4], st[:, 4:5], st[:, 5:6]
        m1, m2, dd, ee, den, qv1, qv2 = st[:, 6:7], st[:, 7:8], st[:, 8:9], st[:, 9:10], st[:, 10:11], st[:, 11:12], st[:, 12:13]
        s1, s2, k1, k2 = st[:, 13:14], st[:, 14:15], st[:, 15:16], st[:, 16:17]
        goh = t8[:, 0, :]
        ex8 = t8[:, 1, :]
        les = t8[:, 2, :]
        oh1 = t8[:, 3, :]
        les2 = t8[:, 4, :]
        oh2 = t8[:, 5, :]
        dv(lambda e: e.tensor_tensor(out=L[:, :], in0=lbank[:, 0:72], in1=bias_b[:, :], op=ALU.add), reads=[lreg, r_c])
        dv(lambda e: e.tensor_reduce(out=gm, in_=G, axis=AX.X, op=ALU.max))
        dv(lambda e: e.tensor_scalar(out=goh, in0=G, scalar1=gm, scalar2=None, op0=ALU.is_equal))
        dv(lambda e: e.tensor_scalar(out=ngm, in0=gm, scalar1=-1.0, scalar2=None, op0=ALU.mult))
        P.op("act", lambda e: e.activation(out=ex8, in_=G, func=AF.Exp, bias=ngm, scale=1.0, accum_out=sg), reads=[r_r], writes=[r_r])
        dv(lambda e: e.reciprocal(out=psel, in_=sg))
        yield
        dv(lambda e: e.tensor_tensor(out=t64[:, :, :], in0=LE, in1=goh.unsqueeze(2).broadcast_to([128, 8, 8]), op=ALU.mult))
        dv(lambda e: e.tensor_reduce(out=les, in_=t64[:, :, :].rearrange("p g e -> p e g"), axis=AX.X, op=ALU.add))
        dv(lambda e: e.tensor_reduce(out=m1, in_=les, axis=AX.X, op=ALU.max))
        dv(lambda e: e.tensor_scalar(out=oh1, in0=les, scalar1=m1, scalar2=None, op0=ALU.is_equal))
        dv(lambda e: e.scalar_tensor_tensor(out=les2, in0=oh1, scalar=-1e30, in1=les, op0=ALU.mult, op1=ALU.add))
        dv(lambda e: e.tensor_reduce(out=m2, in_=les2, axis=AX.X, op=ALU.max))
        dv(lambda e: e.tensor_scalar(out=oh2, in0=les2, scalar1=m2, scalar2=None, op0=ALU.is_equal))
        yield
        dv(lambda e: e.tensor_tensor(out=dd, in0=m2, in1=m1, op=ALU.subtract))
        P.op("act", lambda e: e.activation(out=ee, in_=dd, func=AF.Exp), reads=[r_r], writes=[r_r])
        dv(lambda e: e.tensor_scalar(out=den, in0=ee, scalar1=1.0, scalar2=None, op0=ALU.add))
        dv(lambda e: e.reciprocal(out=qv1, in_=den))
        dv(lambda e: e.tensor_tensor(out=qv2, in0=ee, in1=qv1, op=ALU.mult))
        dv(lambda e: e.tensor_tensor(out=qv1, in0=qv1, in1=psel, op=ALU.mult))
        dv(lambda e: e.tensor_tensor(out=qv2, in0=qv2, in1=psel, op=ALU.mult))
        yield
        dv(lambda e: e.tensor_tensor(out=M1[:, :, :], in0=goh.unsqueeze(2).broadcast_to([128, 8, 8]), in1=oh1.unsqueeze(1).broadcast_to([128, 8, 8]), op=ALU.mult))
        dv(lambda e: e.tensor_tensor(out=M2[:, :, :], in0=goh.unsqueeze(2).broadcast_to([128, 8, 8]), in1=oh2.unsqueeze(1).broadcast_to([128, 8, 8]), op=ALU.mult))
        M1f = M1[:, :, :].rearrange("p g e -> p (g e)")
        M2f = M2[:, :, :].rearrange("p g e -> p (g e)")
        t64f = t64[:, :, :].rearrange("p g e -> p (g e)")
        dv(lambda e: e.tensor_tensor(out=t64f, in0=M1f, in1=M2f, op=ALU.add))
        dv(lambda e: e.tensor_copy(out=Mb[:, :], in_=t64f))
        pbank, preg = next_bank(C)
        P.op("pe", lambda e: e.matmul(out=pbank[:, 0:64], lhsT=trib[:, :], rhs=Mb[:, :], start=True, stop=False), reads=[r_r, r_c], writes=[preg], sig=False)
        P.op("pe", lambda e: e.matmul(out=pbank[:, 0:64], lhsT=onesb[:, :], rhs=mcum_b[:, :], start=False, stop=True), reads=[r_r, r_c, r_mc], writes=[preg], sig=True)
        dv(lambda e: e.tensor_tensor(out=sv[:, :], in0=pbank[:, 0:64], in1=ebase[:, :], op=ALU.add), reads=[preg, r_c])
        yield
        ovf = L[:, 0:64]
        dv(lambda e: e.tensor_single_scalar(out=ovf, in_=pbank[:, 0:64], scalar=CAP - 0.5, op=ALU.is_gt), reads=[preg])
        dv(lambda e: e.tensor_scalar(out=t64f, in0=ovf, scalar1=-1.0, scalar2=1.0, op0=ALU.mult, op1=ALU.add))
        dv(lambda e: e.tensor_tensor(out=sv[:, :], in0=sv[:, :], in1=t64f, op=ALU.mult))
        dv(lambda e: e.scalar_tensor_tensor(out=sv[:, :], in0=ovf, scalar=trash_p[:, 0:1], in1=sv[:, :], op0=ALU.mult, op1=ALU.add), reads=[r_c])
        dv(lambda e: e.tensor_tensor(out=t64f, in0=M1f, in1=M2f, op=ALU.add))
        P.op("dve", lambda e: e.tensor_tensor(out=mcum[:, :], in0=mcum[:, :], in1=t64f, op=ALU.add), reads=[r_r, r_mc], writes=[r_mc])
        P.op("dve", lambda e: e.tensor_copy(out=mcum_b[:, :], in_=mcum[:, :]), reads=[r_mc], writes=[r_mc])
        yield
        dv(lambda e: e.tensor_tensor(out=t64f, in0=M1f, in1=sv[:, :], op=ALU.mult))
        dv(lambda e: e.tensor_reduce(out=s1, in_=t64f, axis=AX.X, op=ALU.add))
        dv(lambda e: e.tensor_tensor(out=t64f, in0=M2f, in1=sv[:, :], op=ALU.mult))
        dv(lambda e: e.tensor_reduce(out=s2, in_=t64f, axis=AX.X, op=ALU.add))
        dv(lambda e: e.tensor_single_scalar(out=k1, in_=s1, scalar=TRASH - 0.5, op=ALU.is_lt))
        dv(lambda e: e.tensor_single_scalar(out=k2, in_=s2, scalar=TRASH - 0.5, op=ALU.is_lt))
        rs_ = C.r_slot[i]
        dv(lambda e: e.tensor_tensor(out=C.gate_w[:, i, 0:1], in0=qv1, in1=k1, op=ALU.mult), writes=[rs_])
        dv(lambda e: e.tensor_tensor(out=C.gate_w[:, i, 1:2], in0=qv2, in1=k2, op=ALU.mult), writes=[rs_])
        dv(lambda e: e.tensor_copy(out=C.slot_i[:, i, 0:1], in_=s1), writes=[rs_])
        dv(lambda e: e.tensor_copy(out=C.slot_i[:, i, 1:2], in_=s2), writes=[rs_])
        for k in range(2):
            P.dma("pool", lambda e: e.indirect_dma_start(out=C.xgd, out_offset=bass.IndirectOffsetOnAxis(ap=C.slot_i[:, i, k:k + 1], axis=0),
                                                         in_=hb[b][:, :], in_offset=None),
                  reads=[r_hb[b], rs_], writes=[C.r_xgt[i]])

    load(0)
    for _ in stageA(0):
        pass
    for i in range(NT):
        interleave(stageB(i), stageA(i + 1) if i + 1 < NT else None, 1)
    if "dbg" in C.debug_out:
        sf = sb(pes, nc, "pf_sf", [128, NT, 2], F32)
        P.op("dve", lambda e: e.tensor_copy(out=sf[:], in_=C.slot_i[:]), reads=C.r_slot, writes=[r_r])
        dview = C.dbg.rearrange("(i p) c -> p i c", p=128)
        P.dma("sp", lambda e: e.dma_start(out=dview[:, :, 0:2], in_=C.gate_w[:]), reads=C.r_slot)
        P.dma("sp", lambda e: e.dma_start(out=dview[:, :, 2:4], in_=sf[:]), reads=[r_r])


def phase_experts(C, pes, layer):
    nc, P = C.nc, C.P
    load_consts(C, pes)
    NB = 2
    NW = 3
    wg = [sb(pes, nc, "ex_wg%d" % i, [128, 16, 512], BF16) for i in range(NW)]
    wu = [sb(pes, nc, "ex_wu%d" % i, [128, 16, 512], BF16) for i in range(NW)]
    wd = [sb(pes, nc, "ex_wd%d" % i, [128, 4, D], BF16) for i in range(NW)]
    r_wg = [Reg() for _ in range(NW)]
    r_wu = [Reg() for _ in range(NW)]
    r_wd = [Reg() for _ in range(NW)]
    Xe = [sb(pes, nc, "ex_X%d" % i, [128, 2, D], BF16) for i in range(NB)]
    r_Xe = [Reg() for _ in range(NB)]
    xT2 = [sb(pes, nc, "ex_xT%d" % i, [128, 16, 256], BF16) for i in range(2)]
    r_xT2 = [Reg() for _ in range(2)]
    sg = [sb(pes, nc, "ex_sg%d" % i, [128, 256], F32) for i in range(2)]
    r_sg = [Reg() for _ in range(2)]
    hT2 = [sb(pes, nc, "ex_hT%d" % i, [128, 4, 256], BF16) for i in range(2)]
    r_hT2 = [Reg() for _ in range(2)]
    Ye = [sb(pes, nc, "ex_Y%d" % i, [128, 2, D], BF16) for i in range(NB)]
    r_Ye = [Reg() for _ in range(NB)]
    zt = sb(pes, nc, "ex_zero", [128, D], BF16)
    r_z = Reg()
    P.op("dve", lambda e: e.memset(zt[:], 0.0), writes=[r_z])
    P.dma("sp", lambda e: e.dma_start(out=C.ygd[NSLOT:NSLOT + 128, :], in_=zt[:]), reads=[r_z], writes=[C.r_ygt])

    nconv = NCONV0 if layer == 0 else NCONV1
    order = list(range(nconv, 64)) + list(range(nconv))
    while C.bg_items and C.bg_items[0][0] == layer:
        bg_issue(C, 1)

    def loadw(k):
        ex = order[k]
        b = k % NW
        if ex < nconv:
            P.dma("sp", lambda e: e.dma_start(out=wg[b][:], in_=C.wgb[layer, ex].rearrange("p (c n) -> p c n", c=16)),
                  reads=[C.r_wb[(layer, ex, 0)]], writes=[r_wg[b]])
            P.dma("sp", lambda e: e.dma_start(out=wu[b][:], in_=C.wub[layer, ex].rearrange("p (c n) -> p c n", c=16)),
                  reads=[C.r_wb[(layer, ex, 1)]], writes=[r_wu[b]])
            P.dma("sp", lambda e: e.dma_start(out=wd[b][:], in_=C.wdb[layer, ex].rearrange("p (c n) -> p c n", c=4)),
                  reads=[C.r_wb[(layer, ex, 2)]], writes=[r_wd[b]])
            return
        for c0 in (0, 8):
            P.dma("pool", lambda e: e.dma_start(out=wg[b][:, c0:c0 + 8, :], in_=C.w_gate[layer, ex, c0 * 128:(c0 + 8) * 128, :].rearrange("(c p) n -> p c n", p=128)),
                  writes=[r_wg[b]])
        for c0 in (0, 8):
            P.dma("pool", lambda e: e.dma_start(out=wu[b][:, c0:c0 + 8, :], in_=C.w_up[layer, ex, c0 * 128:(c0 + 8) * 128, :].rearrange("(c p) n -> p c n", p=128)),
                  writes=[r_wu[b]])
        for c0 in (0, 2):
            P.dma("pool", lambda e: e.dma_start(out=wd[b][:, c0:c0 + 2, :], in_=C.w_down[layer, ex, c0 * 128:(c0 + 2) * 128, :].rearrange("(c p) n -> p c n", p=128)),
                  writes=[r_wd[b]])

    def loadx(k):
        ex = order[k]
        b = k % NB
        P.dma("sp", lambda e: e.dma_start(out=Xe[b][:], in_=C.xgd[ex * CAP:(ex + 1) * CAP, :].rearrange("(t p) d -> p t d", p=128)),
              reads=C.r_xgt, writes=[r_Xe[b]])

    loadw(0)
    loadw(1)
    loadx(0)
    for k in range(64):
        ex = order[k]
        b = k % NB
        w = k % NW
        if k + 2 < 64:
            loadw(k + 2)
        if k + 1 < 64:
            loadx(k + 1)
        xT, r_xT, hT, r_hT = xT2[b], r_xT2[b], hT2[b], r_hT2[b]
        transposes_bf16(C, lambda j: Xe[b][:, j % 2, (j // 2) * 128:(j // 2 + 1) * 128], 32,
                        xT[:, :, :].rearrange("p c (t k) -> p (c t) k", t=2), r_xT, [r_Xe[b]])
        for fc in range(4):
            bank, breg = next_bank(C)
            for c in range(16):
                P.op("pe", lambda e: e.matmul(out=bank[:, 0:256], lhsT=wg[w][:, c, fc * 128:(fc + 1) * 128], rhs=xT[:, c, :], start=(c == 0), stop=(c == 15)),
                     reads=[r_xT, r_wg[w]], writes=[breg], sig=(c == 15))
            for c in range(16):
                P.op("pe", lambda e: e.matmul(out=bank[:, 256:512], lhsT=wu[w][:, c, fc * 128:(fc + 1) * 128], rhs=xT[:, c, :], start=(c == 0), stop=(c == 15)),
                     reads=[r_xT, r_wu[w]], writes=[breg], sig=(c == 15))
            si = fc % 2
            P.op("act", lambda e: e.activation(out=sg[si][:, :], in_=bank[:, 0:256], func=AF.Silu), reads=[breg], writes=[r_sg[si]])
            P.op("dve", lambda e: e.tensor_tensor(out=hT[:, fc, :], in0=bank[:, 256:512], in1=sg[si][:, :], op=ALU.mult), reads=[breg, r_sg[si]], writes=[r_hT])
        for t in range(2):
            for sl in range(4):
                bank, breg = next_bank(C)
                for fc in range(4):
                    P.op("pe", lambda e: e.matmul(out=bank[:, :], lhsT=hT[:, fc, t * 128:(t + 1) * 128], rhs=wd[w][:, fc, sl * 512:(sl + 1) * 512], start=(fc == 0), stop=(fc == 3)),
                         reads=[r_hT, r_wd[w]], writes=[breg], sig=(fc == 3))
                if sl % 2 == 0:
                    P.op("act", lambda e: e.activation(out=Ye[b][:, t, sl * 512:(sl + 1) * 512], in_=bank[:, :], func=AF.Copy), reads=[breg], writes=[r_Ye[b]])
                else:
                    P.op("dve", lambda e: e.tensor_copy(out=Ye[b][:, t, sl * 512:(sl + 1) * 512], in_=bank[:, :]), reads=[breg], writes=[r_Ye[b]])
        P.dma("sp", lambda e: e.dma_start(out=C.ygd[ex * CAP:(ex + 1) * CAP, :].rearrange("(t p) d -> p t d", p=128), in_=Ye[b][:]),
              reads=[r_Ye[b]], writes=[C.r_yg[ex]])


def phase_combine(C, pes, layer):
    nc, P = C.nc, C.P
    if layer == 0:
        Xin, rXin, Xout, rXout = C.x1d, C.r_x1d, C.x2d, C.r_x2d
    else:
        Xin, rXin, Xout, rXout = C.x3d, C.r_x3d, C.out, C.r_out
    NB = 2
    Y1 = [sb(pes, nc, "cb_Y1%d" % i, [128, D], BF16) for i in range(NB)]
    Y2 = [sb(pes, nc, "cb_Y2%d" % i, [128, D], BF16) for i in range(NB)]
    Xt = [sb(pes, nc, "cb_X%d" % i, [128, D], F32) for i in range(NB)]
    Ot = [sb(pes, nc, "cb_O%d" % i, [128, D], F32) for i in range(NB)]
    r_Y1 = [Reg() for _ in range(NB)]
    r_Y2 = [Reg() for _ in range(NB)]
    r_X = [Reg() for _ in range(NB)]
    r_O = [Reg() for _ in range(NB)]
    yregs = list(C.r_yg) + [C.r_ygt]

    def load(i):
        b = i % NB
        P.dma("pool", lambda e: e.indirect_dma_start(out=Y1[b][:, :], out_offset=None, in_=C.ygd,
                                                     in_offset=bass.IndirectOffsetOnAxis(ap=C.slot_i[:, i, 0:1], axis=0)),
              reads=yregs + [C.r_slot[i]], writes=[r_Y1[b]])
        P.dma("pool", lambda e: e.indirect_dma_start(out=Y2[b][:, :], out_offset=None, in_=C.ygd,
                                                     in_offset=bass.IndirectOffsetOnAxis(ap=C.slot_i[:, i, 1:2], axis=0)),
              reads=yregs + [C.r_slot[i]], writes=[r_Y2[b]])
        P.dma("sp", lambda e: e.dma_start(out=Xt[b][:], in_=Xin[i * 128:(i + 1) * 128, :]), reads=[rXin[i]], writes=[r_X[b]])

    load(0)
    for i in range(NT):
        b = i % NB
        if i + 1 < NT:
            load(i + 1)
        P.op("dve", lambda e: e.scalar_tensor_tensor(out=Ot[b][:], in0=Y1[b][:], scalar=C.gate_w[:, i, 0:1], in1=Xt[b][:], op0=ALU.mult, op1=ALU.add),
             reads=[r_Y1[b], r_X[b], C.r_slot[i]], writes=[r_O[b]])
        P.op("dve", lambda e: e.scalar_tensor_tensor(out=Ot[b][:], in0=Y2[b][:], scalar=C.gate_w[:, i, 1:2], in1=Ot[b][:], op0=ALU.mult, op1=ALU.add),
             reads=[r_Y2[b], C.r_slot[i]], writes=[r_O[b]])
        P.dma("sp", lambda e: e.dma_start(out=Xout[i * 128:(i + 1) * 128, :], in_=Ot[b][:]), reads=[r_O[b]], writes=[rXout[i]])


def phase_pool_a(C, pes):
    nc, P = C.nc, C.P
    load_consts(C, pes)
    w_in = sb(pes, nc, "pl_win", [128, 16, D], BF16)
    wgrp = sb(pes, nc, "pl_wg", [128, 16, 512], BF16)
    r_w = Reg()
    load_w_cast(C, w_in, C.pool_w_in, r_w, 16)
    for g in range(4):
        P.dma("pool", lambda e: e.dma_start(out=wgrp[:, g * 4:(g + 1) * 4, :], in_=C.pool_w_group[g].rearrange("(c p) n -> p c n", p=128)), writes=[r_w])
    g_mix = sb(pes, nc, "pl_g", [128, D], F32)
    scale_b = sb(pes, nc, "pl_scale", [128, D], F32)
    bandf = sb(pes, nc, "pl_bandf", [128, 12, 128], F32)
    bandb = sb(pes, nc, "pl_bandb", [128, 12, 128], BF16)
    r_c = Reg()
    load_bcast(C, "sp", g_mix[:], C.mix_norm[1:2, :], D, r_c)
    load_bcast(C, "sp", scale_b[:], C.pool_scale[0:1, :], D, r_c)
    P.dma("sp", lambda e: e.dma_start(out=bandf[:], in_=C.c_band.rearrange("g k a b -> a (g k) b")), writes=[r_c])
    P.op("dve", lambda e: e.tensor_copy(out=bandb[:], in_=bandf[:]), reads=[r_c], writes=[r_c])
    NB = 2
    xt = [sb(pes, nc, "pl_xt%d" % i, [128, D], F32) for i in range(NB)]
    r_xt = [Reg() for _ in range(NB)]
    junk = sb(pes, nc, "pl_junk", [128, D], BF16)
    r_junk = Reg()
    st = sb(pes, nc, "pl_st", [128, 4], F32)
    r_st = Reg()
    hb = sb(pes, nc, "pl_hb", [128, D], BF16)
    r_hb = Reg()
    hT = sb(pes, nc, "pl_hT", [128, 16, 128], BF16)
    r_hT = Reg()
    st2 = [sb(pes, nc, "pl_st%d" % i, [128, 4], F32) for i in range(2)]
    r_st2 = [Reg() for _ in range(2)]
    u = [sb(pes, nc, "pl_u%d" % i, [128, D], BF16) for i in range(3)]
    r_u = [Reg() for _ in range(3)]
    pT = sb(pes, nc, "pl_pT", [128, 16, 128], BF16)
    r_pT = Reg()
    zt = [sb(pes, nc, "pl_z%d" % i, [128, D], BF16) for i in range(NB)]
    r_z = [Reg() for _ in range(NB)]

    def load(i):
        b = i % NB
        P.dma("sp", lambda e: e.dma_start(out=xt[b][:], in_=C.x2d[i * 128:(i + 1) * 128, :]), reads=[C.r_x2d[i]], writes=[r_xt[b]])

    def stageA(i):
        b = i % NB
        if i + 1 < NT:
            load(i + 1)
        X = xt[b]
        st, r_st = st2[b], r_st2[b]
        P.op("act", lambda e: e.activation(out=junk[:], in_=X[:], func=AF.Square, accum_out=st[:, 0:1]), reads=[r_xt[b]], writes=[r_st])
        rstd_from_ss(C, st[:, 0:1], st[:, 1:2], D, r_st, r_st, 1)
        P.op("dve", lambda e: e.scalar_tensor_tensor(out=hb[:], in0=X[:], scalar=st[:, 1:2], in1=g_mix[:], op0=ALU.mult, op1=ALU.mult),
             reads=[r_xt[b], r_st, r_c], writes=[r_hb])
        yield
        transposes_bf16(C, lambda j: hb[:, j * 128:(j + 1) * 128], 16, hT, r_hT, [r_hb])
        yield
        U, Up = u[i % 3], u[(i + 2) % 3]
        rU, rUp = r_u[i % 3], r_u[(i + 2) % 3]
        for s_ in range(4):
            bank, breg = next_bank(C)
            for c in range(16):
                P.op("pe", lambda e: e.matmul(out=bank[:, :], lhsT=hT[:, c, :], rhs=w_in[:, c, s_ * 512:(s_ + 1) * 512], start=(c == 0), stop=(c == 15)),
                     reads=[r_hT, r_w], writes=[breg], sig=(c == 15))
            if s_ % 2 == 0:
                P.op("act", lambda e: e.activation(out=U[:, s_ * 512:(s_ + 1) * 512], in_=bank[:, :], func=AF.Copy), reads=[breg], writes=[rU])
            else:
                P.op("dve", lambda e: e.tensor_copy(out=U[:, s_ * 512:(s_ + 1) * 512], in_=bank[:, :]), reads=[breg], writes=[rU])
            yield

    def stageB(i):
        b = i % NB
        U, Up = u[i % 3], u[(i + 2) % 3]
        rU, rUp = r_u[i % 3], r_u[(i + 2) % 3]
        bg_issue(C, 1 + (i % 2), after=[rU])
        for g0 in range(0, 16, 4):
            bank, breg = next_bank(C)
            for j in range(4):
                c = g0 + j
                gi = c // 4
                kd = 0 if i == 0 else 1
                P.op("pe", lambda e: e.matmul(out=bank[:, j * 128:(j + 1) * 128], lhsT=U[:, c * 128:(c + 1) * 128], rhs=bandb[:, gi * 3 + kd, :], start=True, stop=(i == 0)),
                     reads=[rU, r_c], writes=[breg], sig=(i == 0 and j == 3))
                if i > 0:
                    P.op("pe", lambda e: e.matmul(out=bank[:, j * 128:(j + 1) * 128], lhsT=Up[:, c * 128:(c + 1) * 128], rhs=bandb[:, gi * 3 + 2, :], start=False, stop=True),
                         reads=[rUp, r_c], writes=[breg], sig=(j == 3))
            if (g0 // 4) % 2 == 0:
                P.op("act", lambda e: e.activation(out=pT[:, g0:g0 + 4, :], in_=bank[:, :].rearrange("p (j t) -> p j t", t=128), func=AF.Copy), reads=[breg], writes=[r_pT])
            else:
                P.op("dve", lambda e: e.tensor_copy(out=pT[:, g0:g0 + 4, :], in_=bank[:, :].rearrange("p (j t) -> p j t", t=128)), reads=[breg], writes=[r_pT])
            yield
        for g in range(4):
            bank, breg = next_bank(C)
            for cc in range(4):
                P.op("pe", lambda e: e.matmul(out=bank[:, :], lhsT=pT[:, g * 4 + cc, :], rhs=wgrp[:, g * 4 + cc, :], start=(cc == 0), stop=(cc == 3)),
                     reads=[r_pT, r_w], writes=[breg], sig=(cc == 3))
            P.op("dve", lambda e: e.tensor_tensor(out=zt[b][:, g * 512:(g + 1) * 512], in0=bank[:, :], in1=scale_b[:, g * 512:(g + 1) * 512], op=ALU.mult),
                 reads=[breg, r_c], writes=[r_z[b]])
            yield
        P.dma("sp", lambda e: e.dma_start(out=C.zTd[i * 128:(i + 1) * 128, :], in_=zt[b][:]), reads=[r_z[b]], writes=[C.r_zTd[i]])

    load(0)
    for _ in stageA(0):
        pass
    for i in range(NT):
        interleave(stageB(i), stageA(i + 1) if i + 1 < NT else None, 1)


def host_constants():
    c = {}
    c["c_ident"] = np.eye(128, dtype=np.float32)
    t = np.arange(128)
    c["c_tri"] = (t[:, None] < t[None, :]).astype(np.float32)
    invf = (1.0 / (10000.0 ** (np.arange(0, 64, 2, dtype=np.float32) / 64))).astype(np.float32)
    c["c_invf"] = np.broadcast_to(invf[None, :], (128, 32)).copy()
    band = np.zeros((4, 3, 128, 128), np.float32)
    for gi, w in enumerate((2, 4, 8, 16)):
        for kind in range(3):
            for tt in range(128):
                for j in range(w):
                    src = tt - j
                    if kind == 0:
                        if src >= 0:
                            band[gi, 0, src, tt] += 1.0 / min(tt + 1, w)
                    elif kind == 1:
                        if src >= 0:
                            band[gi, 1, src, tt] += 1.0 / w
                    else:
                        if src < 0:
                            band[gi, 2, 128 + src, tt] += 1.0 / w
            if kind < 2:
                pass
        band[gi, 0] -= np.eye(128, dtype=np.float32)
        band[gi, 1] -= np.eye(128, dtype=np.float32)
    c["c_band"] = band
    c["c_ebase"] = np.broadcast_to((np.arange(64, dtype=np.float32) * CAP)[None, :], (128, 64)).copy()
    return c


def core_inputs(inp, b, consts, with_moe=True):
    m = {}
    m["x"] = np.ascontiguousarray(inp["x"][b])
    m["pos"] = np.ascontiguousarray(np.asarray(inp["positions"][b]).reshape(NT, 128).T)
    m["mix_norm"] = inp["mix_norm"]
    m["mla_w_in"] = inp["mla_w_in"][0]
    m["mla_q_lat_norm"] = inp["mla_q_lat_norm"]
    m["mla_kv_lat_norm"] = inp["mla_kv_lat_norm"]
    m["mla_w_q_up"] = inp["mla_w_q_up"][0]
    m["mla_w_kv_up"] = inp["mla_w_kv_up"][0]
    m["mla_q_norm"] = inp["mla_q_norm"]
    m["mla_k_norm"] = inp["mla_k_norm"]
    m["mla_w_out"] = inp["mla_w_out"][0]
    m["pool_w_in"] = inp["pool_w_in"][0]
    m["pool_w_group"] = inp["pool_w_group"][0]
    m["pool_scale"] = inp["pool_scale"]
    m["pool_w_out"] = inp["pool_w_out"][0]
    m["ffn_norm"] = inp["ffn_norm"]
    m["w_router"] = np.ascontiguousarray(np.concatenate([inp["moe_w_router_group"], inp["moe_w_router_expert"]], axis=2))
    m["b_router"] = np.ascontiguousarray(np.concatenate([inp["moe_b_router_group"], np.asarray(inp["moe_b_router_expert"]).reshape(2, 64)], axis=1))
    if with_moe:
        m["moe_w_gate"] = inp["moe_w_gate"]
        m["moe_w_up"] = inp["moe_w_up"]
        m["moe_w_down"] = inp["moe_w_down"]
    m.update(consts)
    return {k: np.ascontiguousarray(np.asarray(v)) for k, v in m.items()}


_CACHE = {}


def kernel(**inputs):
    inp = {k: np.asarray(v) for k, v in inputs.items()}
    consts = host_constants()
    if "nc" not in _CACHE:
        _CACHE["nc"] = build_program()
    nc = _CACHE["nc"]
    in_maps = [core_inputs(inp, b, consts) for b in range(8)]
    res = run_bass_kernel_spmd(nc, in_maps, core_ids=list(range(8)))
    return np.stack([np.asarray(res.results[b]["out"]) for b in range(8)], axis=0).astype(np.float32)
```
